# Optimizing a Trainium2 kernel written in Bass

```python
import jax, jax.numpy as jnp
from jax import lax
import numpy as np

D_MODEL = 1024
BATCH = 2
SEQ = 16384
DEPTH = 4

GRID_W = 64
CTX_LEN = 256
N_MIXERS = 2
POOL_WINDOWS = (2, 4, 8, 16)
N_POOL_GROUPS = 4
POOL_GROUP_DIM = D_MODEL // N_POOL_GROUPS
HEAD_DIM = 64
N_Q_HEADS = D_MODEL // HEAD_DIM
N_KV_HEADS = N_Q_HEADS // 4
Q_PER_KV = N_Q_HEADS // N_KV_HEADS
QKV_DIM = (N_Q_HEADS + 2 * N_KV_HEADS) * HEAD_DIM
WINDOW = 128
BLOCK_Q = 128
ROPE_THETA = 10000.0
N_EXPERTS = 16
EC_CAPACITY_FACTOR = 2
D_EXPERT = 2048
NORM_EPS = 1e-6
NEG_INF = -1e30

kernel_name = "hybrid_pool_swa_ecmoe_diffusion"


def rmsnorm(x, g):
    xf = x.astype(jnp.float32)
    y = xf * lax.rsqrt(jnp.mean(xf * xf, axis=-1, keepdims=True) + NORM_EPS)
    return (y * g.astype(jnp.float32)).astype(x.dtype)


def ada_params(cvec, w, b):
    m = jax.nn.silu(cvec) @ w + b
    return jnp.split(m, 6, axis=-1)


def modulate(h, shift, scale):
    return h * (1 + scale) + shift


def rope_2d(x):
    B, L, H, Dh = x.shape
    rows = L // GRID_W
    row = jnp.repeat(jnp.arange(rows), GRID_W).astype(jnp.float32)
    col = jnp.tile(jnp.arange(GRID_W), rows).astype(jnp.float32)
    n_freq = Dh // 4
    inv = ROPE_THETA ** (-jnp.arange(n_freq, dtype=jnp.float32) / n_freq)
    ang = jnp.concatenate([row[:, None] * inv, col[:, None] * inv], axis=-1)
    cos = jnp.cos(ang)[None, :, None, :]
    sin = jnp.sin(ang)[None, :, None, :]
    xp = x.astype(jnp.float32).reshape(B, L, H, Dh // 2, 2)
    x0, x1 = xp[..., 0], xp[..., 1]
    out = jnp.stack([x0 * cos - x1 * sin, x0 * sin + x1 * cos], axis=-1)
    return out.reshape(B, L, H, Dh).astype(x.dtype)


def pool_mix(h, w_pool, scale):
    B, L, D = h.shape
    hf = h.astype(jnp.float32)
    cs = jnp.concatenate([jnp.zeros((B, 1, D), jnp.float32), jnp.cumsum(hf, axis=1)], axis=1)
    t = jnp.arange(L)
    groups = []
    for g, w in enumerate(POOL_WINDOWS):
        half = w // 2
        lo = jnp.clip(t - half, 0, L)
        hi = jnp.clip(t + half, 0, L)
        sl = slice(g * POOL_GROUP_DIM, (g + 1) * POOL_GROUP_DIM)
        csg = cs[..., sl]
        mean = (csg[:, hi] - csg[:, lo]) / (hi - lo).astype(jnp.float32)[None, :, None]
        groups.append(mean - hf[..., sl])
    p = jnp.stack(groups, axis=2)
    y = jnp.einsum('blgc,gcd->blgd', p, w_pool.astype(jnp.float32)).reshape(B, L, D)
    return (y * scale.astype(jnp.float32)).astype(h.dtype)


def project_qkv(h, w_qkv, q_norm, k_norm):
    B, L, _ = h.shape
    proj = h @ w_qkv
    nq = N_Q_HEADS * HEAD_DIM
    nk = N_KV_HEADS * HEAD_DIM
    q = rmsnorm(proj[..., :nq].reshape(B, L, N_Q_HEADS, HEAD_DIM), q_norm)
    k = rmsnorm(proj[..., nq:nq + nk].reshape(B, L, N_KV_HEADS, HEAD_DIM), k_norm)
    v = proj[..., nq + nk:].reshape(B, L, N_KV_HEADS, HEAD_DIM)
    return q, k, v


def project_kv(h, w_qkv, k_norm):
    B, L, _ = h.shape
    nq = N_Q_HEADS * HEAD_DIM
    nk = N_KV_HEADS * HEAD_DIM
    proj = h @ w_qkv[:, nq:]
    k = rmsnorm(proj[..., :nk].reshape(B, L, N_KV_HEADS, HEAD_DIM), k_norm)
    v = proj[..., nk:].reshape(B, L, N_KV_HEADS, HEAD_DIM)
    return k, v


def attn_latent(q, k, v, kc, vc, sink):
    B, L, _, _ = q.shape
    Lc = kc.shape[1]
    nb = L // BLOCK_Q
    band = BLOCK_Q + 2 * WINDOW
    scale = HEAD_DIM ** -0.5
    kpad = jnp.pad(k, ((0, 0), (WINDOW, WINDOW), (0, 0), (0, 0)))
    vpad = jnp.pad(v, ((0, 0), (WINDOW, WINDOW), (0, 0), (0, 0)))
    sink_b = jnp.broadcast_to(sink.astype(jnp.float32).reshape(N_KV_HEADS, Q_PER_KV)[None, :, :, None, None],
                              (B, N_KV_HEADS, Q_PER_KV, BLOCK_Q, 1))

    def block(bi):
        start = bi * BLOCK_Q
        qb = lax.dynamic_slice_in_dim(q, start, BLOCK_Q, axis=1).reshape(B, BLOCK_Q, N_KV_HEADS, Q_PER_KV, HEAD_DIM)
        kb = lax.dynamic_slice_in_dim(kpad, start, band, axis=1)
        vb = lax.dynamic_slice_in_dim(vpad, start, band, axis=1)
        qi = start + jnp.arange(BLOCK_Q)
        kj = start - WINDOW + jnp.arange(band)
        mask = (kj[None, :] >= 0) & (kj[None, :] < L) & (jnp.abs(qi[:, None] - kj[None, :]) <= WINDOW)
        s_loc = jnp.einsum('bqkgd,bskd->bkgqs', qb, kb).astype(jnp.float32) * scale
        s_loc = jnp.where(mask[None, None, None], s_loc, NEG_INF)
        s_ctx = jnp.einsum('bqkgd,bskd->bkgqs', qb, kc).astype(jnp.float32) * scale
        p = jax.nn.softmax(jnp.concatenate([s_loc, s_ctx, sink_b], axis=-1), axis=-1).astype(v.dtype)
        o = (jnp.einsum('bkgqs,bskd->bqkgd', p[..., :band], vb)
             + jnp.einsum('bkgqs,bskd->bqkgd', p[..., band:band + Lc], vc))
        return o.reshape(B, BLOCK_Q, N_Q_HEADS * HEAD_DIM)

    out = lax.map(block, jnp.arange(nb))
    return jnp.transpose(out, (1, 0, 2, 3)).reshape(B, L, N_Q_HEADS * HEAD_DIM)


def attn_context(qc, kc, vc, sink):
    B, Lc, _, _ = qc.shape
    scale = HEAD_DIM ** -0.5
    qg = qc.reshape(B, Lc, N_KV_HEADS, Q_PER_KV, HEAD_DIM)
    s = jnp.einsum('bqkgd,bskd->bkgqs', qg, kc).astype(jnp.float32) * scale
    sink_b = jnp.broadcast_to(sink.astype(jnp.float32).reshape(N_KV_HEADS, Q_PER_KV)[None, :, :, None, None],
                              (B, N_KV_HEADS, Q_PER_KV, Lc, 1))
    p = jax.nn.softmax(jnp.concatenate([s, sink_b], axis=-1), axis=-1).astype(vc.dtype)
    o = jnp.einsum('bkgqs,bskd->bqkgd', p[..., :Lc], vc)
    return o.reshape(B, Lc, N_Q_HEADS * HEAD_DIM)


def ec_moe(h, w_router, w_gate, w_up, w_down):
    B, N, D = h.shape
    C = EC_CAPACITY_FACTOR * N // N_EXPERTS
    aff = jax.nn.softmax(jnp.einsum('bnd,de->bne', h, w_router).astype(jnp.float32), axis=-1)
    vals, idx = lax.top_k(jnp.swapaxes(aff, 1, 2), C)
    xs = jax.vmap(lambda hb, ib: hb[ib])(h, idx)
    a = jnp.einsum('becd,edf->becf', xs, w_gate)
    u = jnp.einsum('becd,edf->becf', xs, w_up)
    y = jnp.einsum('becf,efd->becd', jax.nn.silu(a) * u, w_down) * vals[..., None].astype(h.dtype)
    out = jax.vmap(lambda ib, yb: jnp.zeros((N, D), yb.dtype).at[ib.reshape(-1)].add(yb.reshape(-1, D)))(idx, y)
    return out


def setup_inputs(seed: int = 0) -> dict:
    key = jax.random.key(seed)
    ks = jax.random.split(key, 20)
    n_pool = (DEPTH + 1) // 2
    n_attn = DEPTH // 2
    D = D_MODEL
    nrm = jax.random.normal
    f32 = jnp.float32
    return {
        "x": nrm(ks[0], (BATCH, SEQ, D), f32),
        "c": nrm(ks[1], (BATCH, D), f32),
        "ctx": nrm(ks[2], (BATCH, CTX_LEN, D), f32),
        "c_ctx": nrm(ks[3], (D,), f32),
        "w_ada": nrm(ks[4], (DEPTH, D, 6 * D), f32) * (0.5 * D ** -0.5),
        "b_ada": nrm(ks[5], (DEPTH, 6 * D), f32) * 0.02,
        "norm1_g": 1.0 + 0.05 * nrm(ks[6], (DEPTH, D), f32),
        "norm2_g": 1.0 + 0.05 * nrm(ks[7], (DEPTH, D), f32),
        "pool_w": nrm(ks[8], (n_pool, N_POOL_GROUPS, POOL_GROUP_DIM, POOL_GROUP_DIM), f32) * POOL_GROUP_DIM ** -0.5,
        "pool_scale": 1.0 + 0.05 * nrm(ks[9], (n_pool, D), f32),
        "attn_w_qkv": nrm(ks[10], (n_attn, D, QKV_DIM), f32) * D ** -0.5,
        "attn_w_o": nrm(ks[11], (n_attn, N_Q_HEADS * HEAD_DIM, D), f32) * (N_Q_HEADS * HEAD_DIM) ** -0.5,
        "attn_q_norm": 1.0 + 0.05 * nrm(ks[12], (n_attn, HEAD_DIM), f32),
        "attn_k_norm": 1.0 + 0.05 * nrm(ks[13], (n_attn, HEAD_DIM), f32),
        "attn_sink": 0.5 * nrm(ks[14], (n_attn, N_Q_HEADS), f32),
        "router_w": nrm(ks[15], (DEPTH, D, N_EXPERTS), f32) * D ** -0.5,
        "exp_w_gate": nrm(ks[16], (DEPTH, N_EXPERTS, D, D_EXPERT), f32) * D ** -0.5,
        "exp_w_up": nrm(ks[17], (DEPTH, N_EXPERTS, D, D_EXPERT), f32) * D ** -0.5,
        "exp_w_down": nrm(ks[18], (DEPTH, N_EXPERTS, D_EXPERT, D), f32) * D_EXPERT ** -0.5,
    }


def reference(x, c, ctx, c_ctx, w_ada, b_ada, norm1_g, norm2_g, pool_w, pool_scale,
              attn_w_qkv, attn_w_o, attn_q_norm, attn_k_norm, attn_sink,
              router_w, exp_w_gate, exp_w_up, exp_w_down):
    for i in range(DEPTH):
        update_ctx = i < DEPTH - 1
        j = i // N_MIXERS
        is_pool = (i % N_MIXERS) == 0
        sh1, sc1, g1, sh2, sc2, g2 = [m[:, None, :] for m in ada_params(c, w_ada[i], b_ada[i])]
        csh1, csc1, cg1, csh2, csc2, cg2 = [m[None, None, :] for m in ada_params(c_ctx, w_ada[i], b_ada[i])]

        h = modulate(rmsnorm(x, norm1_g[i]), sh1, sc1)
        if is_pool:
            y = pool_mix(h, pool_w[j], pool_scale[j])
            if update_ctx:
                hc = modulate(rmsnorm(ctx, norm1_g[i]), csh1, csc1)
                yc = pool_mix(hc, pool_w[j], pool_scale[j])
        else:
            hc = modulate(rmsnorm(ctx, norm1_g[i]), csh1, csc1)
            q, k, v = project_qkv(h, attn_w_qkv[j], attn_q_norm[j], attn_k_norm[j])
            q = rope_2d(q)
            k = rope_2d(k)
            if update_ctx:
                qc, kc, vc = project_qkv(hc, attn_w_qkv[j], attn_q_norm[j], attn_k_norm[j])
                yc = attn_context(qc, kc, vc, attn_sink[j]) @ attn_w_o[j]
            else:
                kc, vc = project_kv(hc, attn_w_qkv[j], attn_k_norm[j])
            y = attn_latent(q, k, v, kc, vc, attn_sink[j]) @ attn_w_o[j]

        x = x + g1 * y
        h2 = modulate(rmsnorm(x, norm2_g[i]), sh2, sc2)
        x = x + g2 * ec_moe(h2, router_w[i], exp_w_gate[i], exp_w_up[i], exp_w_down[i])

        if update_ctx:
            ctx = ctx + cg1 * yc
            hc2 = modulate(rmsnorm(ctx, norm2_g[i]), csh2, csc2)
            ctx = ctx + cg2 * ec_moe(hc2, router_w[i], exp_w_gate[i], exp_w_up[i], exp_w_down[i])
    return x
```

```python
import numpy as np
from contextlib import ExitStack
import concourse.bass as bass
import concourse.mybir as mybir
from concourse.bass_utils import run_bass_kernel_spmd

F32 = mybir.dt.float32
BF16 = mybir.dt.bfloat16
I32 = mybir.dt.int32
U32 = mybir.dt.uint32
AF = mybir.ActivationFunctionType
ALU = mybir.AluOpType
AX = mybir.AxisListType

D = 1024
CTX = 256
NE = 16
DE = 2048
RW = 1088
EPS = 1e-6
POOL_W = (2, 4, 8, 16)
BIG = 1.0e6


class Sem:
    def __init__(self, nc, name):
        self.h = nc.alloc_semaphore(name)
        self.count = 0


class Buf:
    def __init__(self, name, const=False):
        self.name = name
        self.last_w = None
        self.readers = []
        self.const = const


class Tl:
    def __init__(self, t, name, const=False):
        self.t = t
        self.b = Buf(name, const)

    def __getitem__(self, k):
        return self.t[k]


def _b(x):
    return x.b if isinstance(x, Tl) else x


class Sched:
    def __init__(self, nc):
        self.nc = nc
        self.eng = {"pe": nc.tensor, "act": nc.scalar, "dve": nc.vector, "pool": nc.gpsimd, "sp": nc.sync}
        self.esem = {k: Sem(nc, "es_" + k) for k in ["pe", "act", "dve", "pool"]}
        self.seen = {k: {} for k in self.eng}
        self.dsems = []
        self.named = {}
        self.nops = 0

    def dsem(self, name):
        base = name.split("@")[0]
        if base in self.named:
            return self.named[base]
        s = Sem(self.nc, base)
        self.dsems.append(s)
        self.named[base] = s
        return s

    def _wait(self, engname, deps):
        best = {}
        for s, v in deps:
            if best.get(s, 0) < v:
                best[s] = v
        for s, v in best.items():
            if self.seen[engname].get(s, 0) >= v:
                continue
            self.eng[engname].wait_ge(s.h, v)
            self.seen[engname][s] = v

    def op(self, engname, fn, reads=(), writes=(), dma=None):
        deps = []
        for b in reads:
            b = _b(b)
            if b.last_w is not None:
                deps.append(b.last_w)
        for b in writes:
            b = _b(b)
            if b.last_w is not None:
                deps.append(b.last_w)
            deps.extend(b.readers)
        if engname == "pe":
            deps = [d for d in deps if d[0] is not self.esem["pe"]]
        self._wait(engname, deps)
        ins = fn(self.eng[engname])
        self.nops += 1
        if dma is not None:
            dma.count += 16
            ins.then_inc(dma.h, 16)
            tok = (dma, dma.count)
        else:
            s = self.esem[engname]
            s.count += 1
            ins.then_inc(s.h, 1)
            tok = (s, s.count)
        for b in reads:
            b = _b(b)
            if not b.const:
                b.readers.append(tok)
        for b in writes:
            b = _b(b)
            b.last_w = tok
            b.readers = []
        return tok

    def barrier(self):
        toks = [(s, s.count) for s in self.esem.values() if s.count > 0]
        toks += [(d, d.count) for d in self.dsems if d.count > 0]
        for e in self.eng:
            self._wait(e, toks)


def band_consts():
    out = np.zeros((3, 3, 128, 4, 128), np.float32)
    for v in range(3):
        for g, w in enumerate(POOL_W):
            half = w // 2
            for t in range(128):
                lo, hi = t - half, t + half
                lo_c = max(lo, 0) if v == 0 else lo
                hi_c = min(hi, 128) if v == 2 else hi
                cnt = hi_c - lo_c
                for tp in range(lo_c, hi_c):
                    if tp < 0:
                        out[v, 0, 128 + tp, g, t] += 1.0 / cnt
                    elif tp >= 128:
                        out[v, 2, tp - 128, g, t] += 1.0 / cnt
                    else:
                        out[v, 1, tp, g, t] += 1.0 / cnt
                out[v, 1, t, g, t] -= 1.0
    return out


def build(L, depth, n_pool, n_attn):
    NT = L // 128
    NTT = NT + 2
    CL = L // 8
    NS = CL // 128 + 1
    NSL = CL + 128
    TT = []
    s0 = 0
    while s0 < NSL:
        n = min(512, NSL - s0)
        TT.append((s0, n))
        s0 += n
    nc = bass.Bass("TRN2", target_bir_lowering=False)

    def din(name, shape, dt=F32):
        return nc.dram_tensor(name, list(shape), dt, kind="ExternalInput").ap()

    x_in = din("x", [L, D]); ctx_in = din("ctx", [CTX, D]); cvT = din("cvT", [128, 8, 2])
    w_ada = din("w_ada", [depth, D, 6 * D]); b_ada = din("b_ada", [depth, 6 * D])
    n1g = din("norm1_g", [depth, D]); n2g = din("norm2_g", [depth, D])
    pool_w = din("pool_w", [max(n_pool, 1), 4, 256, 256]); pool_sc = din("pool_scale", [max(n_pool, 1), D])
    wqkv = din("attn_w_qkv", [max(n_attn, 1), D, 1536]); wo = din("attn_w_o", [max(n_attn, 1), D, D])
    gqk_in = din("gqk", [max(n_attn, 1), 1280]); gq_in = din("attn_q_norm", [max(n_attn, 1), 64]); gk_in = din("attn_k_norm", [max(n_attn, 1), 64])
    sink_in = din("attn_sink", [max(n_attn, 1), 16])
    wr_in = din("router_w", [depth, D, NE])
    wg_in = din("exp_w_gate", [depth, NE, D, DE]); wu_in = din("exp_w_up", [depth, NE, D, DE]); wd_in = din("exp_w_down", [depth, NE, DE, D])
    c_ident = din("c_ident", [128, 128]); c_lstrict = din("c_lstrict", [128, 128]); c_masks = din("c_masks", [2, 128, 512])
    c_band = din("c_band", [3, 3, 128, 4, 128]); c_tokid = din("c_tokid", [128, NTT], I32); c_rope = din("c_rope", [NTT * 128, 64])
    out_d = nc.dram_tensor("out", [L, D], F32, kind="ExternalOutput").ap()
    X = nc.dram_tensor("Xres", [NTT * 128, D], F32, kind="Internal").ap()
    H2 = nc.dram_tensor("H2rows", [NTT * 128, RW], BF16, kind="Internal").ap()
    XS = [nc.dram_tensor("XS%d" % i, [NSL, RW], BF16, kind="Internal").ap() for i in range(3)]
    bX = Buf("X"); bH2 = Buf("H2"); bXS = [Buf("XS0"), Buf("XS1"), Buf("XS2")]; bOut = Buf("out")

    S = Sched(nc)
    op = S.op
    with ExitStack() as top:
        uid = [0]

        def sb(es, name, shape, dt, const=False):
            uid[0] += 1
            name = "%s_u%d" % (name, uid[0])
            return Tl(es.enter_context(nc.sbuf_tensor(name, list(shape), dt)), name, const)

        top.enter_context(nc.allow_low_precision(reason="bf16 0/1 masks and small integer counts are exact; bf16 matmul operands by design"))
        PF = [Tl(top.enter_context(nc.psum_tensor("pf%d" % i, [128, 512], F32)), "pf%d" % i) for i in range(7)]
        PT = Tl(top.enter_context(nc.psum_tensor("ptb", [128, 1024], BF16)), "ptb")
        d_c = S.dsem("d_c")
        reg_lat = nc.gpsimd.alloc_register("bnd_lat"); nc.gpsimd.reg_mov(reg_lat, CL - 1)
        reg_ctx = nc.gpsimd.alloc_register("bnd_ctx"); nc.gpsimd.reg_mov(reg_ctx, CL + 31)
        identf = sb(top, "identf", [128, 128], F32, True)
        identb = sb(top, "identb", [128, 128], BF16, True)
        lstr = sb(top, "lstr", [128, 128], BF16, True)
        onesb = sb(top, "onesb", [128, 128], BF16, True)
        ones1 = sb(top, "ones1", [1, 128], F32, True)
        tokid = sb(top, "tokid", [128, NTT], I32, True)
        cvec = sb(top, "cvec", [128, 8, 2], F32, True)
        scT = sb(top, "scT", [128, 8, 2], F32, True)
        G2 = [sb(top, "G2_%d" % r, [128, D], F32) for r in range(2)]
        POSI = sb(top, "POSI", [128, NTT, NE], I32)
        with ExitStack() as es0:
            stg = sb(es0, "cstg", [128, 128], F32)
            zt = sb(es0, "zt", [128, RW], BF16)
            op("sp", lambda e: e.dma_start(out=identf[:], in_=c_ident[:, :]), writes=[identf], dma=d_c)
            op("sp", lambda e: e.dma_start(out=stg[:], in_=c_lstrict[:, :]), writes=[stg], dma=d_c)
            op("sp", lambda e: e.dma_start(out=tokid[:], in_=c_tokid[:, :]), writes=[tokid], dma=d_c)
            op("sp", lambda e: e.dma_start(out=cvec[:], in_=cvT[:, :, :]), writes=[cvec], dma=d_c)
            S.barrier()
            op("dve", lambda e: e.tensor_copy(out=identb[:], in_=identf[:]), reads=[identf], writes=[identb])
            op("dve", lambda e: e.tensor_copy(out=lstr[:], in_=stg[:]), reads=[stg], writes=[lstr])
            op("dve", lambda e: e.memset(onesb[:], 1.0), writes=[onesb])
            op("dve", lambda e: e.memset(ones1[:], 1.0), writes=[ones1])
            op("act", lambda e: e.activation(out=scT[:], in_=cvec[:], func=AF.Silu), reads=[cvec], writes=[scT])
            op("dve", lambda e: e.memset(zt[:], 0.0), writes=[zt])
            for i in range(3):
                for r0 in range(0, NSL, 128):
                    op("sp", lambda e: e.dma_start(out=XS[i][r0:r0 + 128, :], in_=zt[:]), reads=[zt], writes=[bXS[i]], dma=d_c)
            CH = 1024
            for r0 in range(0, L, CH):
                n = min(CH, L - r0)
                op("sp", lambda e: e.dma_start(out=X[r0:r0 + n, :], in_=x_in[r0:r0 + n, :]), writes=[bX], dma=d_c)
            op("sp", lambda e: e.dma_start(out=X[L:L + CTX, :], in_=ctx_in[:, :]), writes=[bX], dma=d_c)
            S.barrier()

        for l in range(depth):
            is_pool = (l % 2 == 0)
            jm = l // 2
            last = (l == depth - 1)
            upd_ctx = not last
            with ExitStack() as esL:
                V = [dict(), dict()]
                names = ["A1", "B1", "G1", "A2", "B2"]
                esT = ExitStack()
                esL.enter_context(esT)
                for r in range(2):
                    for nm in names:
                        V[r][nm] = sb(esT, "V%d%s" % (r, nm), [128, D], F32)
                    V[r]["G2"] = G2[r]
                AFF = sb(esT, "AFF", [128, NTT, NE], F32)
                if not is_pool:
                    GQK = sb(esT, "GQK", [128, 1280], F32)
                    SK = sb(esT, "SK", [128, 17], F32)
                with ExitStack() as esA:
                    d_w = [S.dsem("d_adaw%d" % i) for i in range(2)]
                    d_s = S.dsem("d_adas")
                    wst = [sb(esA, "adaw%d" % i, [128, 8, 512], F32) for i in range(2)]
                    mrow = [sb(esA, "mrow%d" % r, [1, 6 * D], F32) for r in range(2)]
                    brow = sb(esA, "brow", [1, 6 * D], F32)
                    g1row = sb(esA, "g1row", [1, D], F32); g2row = sb(esA, "g2row", [1, D], F32); psrow = sb(esA, "psrow", [1, D], F32)
                    vrow = sb(esA, "vrow", [1, D], F32)
                    op("sp", lambda e: e.dma_start(out=brow[:], in_=b_ada[l:l + 1, :]), writes=[brow], dma=d_s)
                    op("sp", lambda e: e.dma_start(out=g1row[:], in_=n1g[l:l + 1, :]), writes=[g1row], dma=d_s)
                    op("sp", lambda e: e.dma_start(out=g2row[:], in_=n2g[l:l + 1, :]), writes=[g2row], dma=d_s)
                    if is_pool:
                        op("sp", lambda e: e.dma_start(out=psrow[:], in_=pool_sc[jm:jm + 1, :]), writes=[psrow], dma=d_s)
                    if not is_pool:
                        qrow = sb(esA, "qrow", [1, 1280], F32)
                        grow = sb(esA, "grow", [1, 128], F32)
                        srow = sb(esA, "srow", [1, 32], F32)
                        op("sp", lambda e: e.dma_start(out=qrow[:], in_=gqk_in[jm:jm + 1, :]), writes=[qrow], dma=d_s)
                        op("sp", lambda e: e.dma_start(out=grow[0:1, 0:64], in_=gq_in[jm:jm + 1, :]), writes=[grow], dma=d_s)
                        op("sp", lambda e: e.dma_start(out=grow[0:1, 64:128], in_=gk_in[jm:jm + 1, :]), writes=[grow], dma=d_s)
                        op("sp", lambda e: e.dma_start(out=srow[0:1, 1:17], in_=sink_in[jm:jm + 1, :]), writes=[srow], dma=d_s)
                    S.barrier()
                    for nb in range(12):
                        w = wst[nb % 2]
                        op("sp", lambda e: e.dma_start(out=w[:], in_=w_ada[l, :, nb * 512:(nb + 1) * 512].rearrange("(k p) f -> p k f", p=128)), writes=[w], dma=d_w[nb % 2])
                        for r in range(2):
                            pm = PF[r]
                            for kc in range(8):
                                op("pe", lambda e: e.matmul(pm[0:1, :], lhsT=scT[:, kc, r:r + 1], rhs=w[:, kc, :], start=(kc == 0), stop=(kc == 7)), reads=[w, scT], writes=[pm])
                            op("dve", lambda e: e.tensor_tensor(out=mrow[r][0:1, nb * 512:(nb + 1) * 512], in0=pm[0:1, :], in1=brow[0:1, nb * 512:(nb + 1) * 512], op=ALU.add), reads=[pm, brow], writes=[mrow[r]])

                    def bcast(dst, row_ap, row_t, width=D):
                        for h0 in range(0, width, 512):
                            n = min(512, width - h0)
                            pb = PF[2 + (h0 // 512) % 2]
                            op("pe", lambda e: e.matmul(pb[:, 0:n], lhsT=ones1[0:1, :], rhs=row_ap[0:1, h0:h0 + n], start=True, stop=True), reads=[row_t, ones1], writes=[pb])
                            op("act", lambda e: e.copy(out=dst[:, h0:h0 + n], in_=pb[:, 0:n]), reads=[pb], writes=[dst])

                    for r in range(2):
                        m = mrow[r]
                        seg = lambda i: m[0:1, i * D:(i + 1) * D]
                        op("dve", lambda e: e.scalar_tensor_tensor(out=vrow[:], in0=seg(1), scalar=1.0, in1=g1row[:], op0=ALU.add, op1=ALU.mult), reads=[m, g1row], writes=[vrow])
                        bcast(V[r]["A1"], vrow, vrow)
                        bcast(V[r]["B1"], seg(0), m)
                        if is_pool:
                            op("dve", lambda e: e.tensor_tensor(out=vrow[:], in0=seg(2), in1=psrow[:], op=ALU.mult), reads=[m, psrow], writes=[vrow])
                            bcast(V[r]["G1"], vrow, vrow)
                        else:
                            bcast(V[r]["G1"], seg(2), m)
                        op("dve", lambda e: e.scalar_tensor_tensor(out=vrow[:], in0=seg(4), scalar=1.0, in1=g2row[:], op0=ALU.add, op1=ALU.mult), reads=[m, g2row], writes=[vrow])
                        bcast(V[r]["A2"], vrow, vrow)
                        bcast(V[r]["B2"], seg(3), m)
                        bcast(V[r]["G2"], seg(5), m)
                    if not is_pool:
                        bcast(GQK, qrow, qrow, 1280)
                        op("dve", lambda e: e.tensor_reduce(out=srow[0:1, 20:22], in_=grow[0:1, :].rearrange("p (a b) -> p a b", b=64), axis=AX.X, op=ALU.max, apply_absolute_value=True), reads=[grow], writes=[srow])
                        op("dve", lambda e: e.scalar_tensor_tensor(out=srow[0:1, 0:1], in0=srow[0:1, 20:21], scalar=-8.0, in1=srow[0:1, 21:22], op0=ALU.mult, op1=ALU.mult), reads=[srow], writes=[srow])
                        op("dve", lambda e: e.tensor_scalar(out=srow[0:1, 1:17], in0=srow[0:1, 1:17], scalar1=srow[0:1, 0:1], scalar2=None, op0=ALU.add), reads=[srow], writes=[srow])
                        pb = PF[2]
                        op("pe", lambda e: e.matmul(pb[:, 0:17], lhsT=ones1[0:1, :], rhs=srow[0:1, 0:17], start=True, stop=True), reads=[srow, ones1], writes=[pb])
                        op("dve", lambda e: e.tensor_copy(out=SK[:, 0:1], in_=pb[:, 0:1]), reads=[pb], writes=[SK])
                        op("act", lambda e: e.activation(out=SK[:, 1:17], in_=pb[:, 1:17], func=AF.Exp), reads=[pb], writes=[SK])
                    S.barrier()

                with ExitStack() as esP:
                    d_x = [S.dsem("d_x%d" % i) for i in range(2)]
                    d_o = [S.dsem("d_o%d" % i) for i in range(2)]
                    d_r = [S.dsem("d_r%d" % i) for i in range(2)]
                    d_m = S.dsem("d_m")
                    xt = [sb(esP, "xt%d" % i, [128, D], F32) for i in range(2)]
                    sqj = sb(esP, "sqj", [128, 1280], F32)
                    st1 = sb(esP, "st1", [128, 4], F32)
                    hf = sb(esP, "hf", [128, D], F32)
                    xm = [sb(esP, "xm0", [128, D], F32)] * 2
                    h2f = sb(esP, "h2f", [128, D], F32)
                    h2T = sb(esP, "h2T", [128, 8, 128], F32)
                    rowt = [sb(esP, "rowt%d" % i, [128, RW], BF16) for i in range(2)]
                    lg = sb(esP, "lg", [128, 64], F32)
                    wr = sb(esP, "wr", [128, 8, NE], F32, True)
                    op("sp", lambda e: e.dma_start(out=wr[:], in_=wr_in[l].rearrange("(k p) e -> p k e", p=128)), writes=[wr], dma=d_m)
                    NSLOT = 5
                    slot = lambda j: (j % 3) if j < NT else 3 + (j - NT)
                    if is_pool:
                        hb = [sb(esP, "hb%d" % i, [128, D], BF16) for i in range(NSLOT)]
                        pw = sb(esP, "pw", [128, 8, 256], BF16, True)
                        band = sb(esP, "band", [128, 9, 4, 128], BF16, True)
                        pT = sb(esP, "pT", [128, 8, 128], BF16)
                        with ExitStack() as esW:
                            wtmp = sb(esW, "wtmp", [128, 9 * 4 * 128], F32)
                            op("sp", lambda e: e.dma_start(out=wtmp[:, 0:2048].rearrange("p (k f) -> p k f", f=256), in_=pool_w[jm].rearrange("g (c p) f -> p (g c) f", p=128)), writes=[wtmp], dma=d_m)
                            op("dve", lambda e: e.tensor_copy(out=pw[:].rearrange("p k f -> p (k f)"), in_=wtmp[:, 0:2048]), reads=[wtmp], writes=[pw])
                            op("sp", lambda e: e.dma_start(out=wtmp[:].rearrange("p (v g t) -> p v g t", g=4, t=128), in_=c_band.rearrange("a b p g t -> p (a b) g t")), writes=[wtmp], dma=d_m)
                            op("dve", lambda e: e.tensor_copy(out=band[:].rearrange("p v g t -> p (v g t)"), in_=wtmp[:]), reads=[wtmp], writes=[band])
                            S.barrier()
                    else:
                        kT = [sb(esP, "kT%d" % i, [64, 4, 128], BF16) for i in range(NSLOT)]
                        vv = [sb(esP, "vv%d" % i, [128, 4, 65], BF16) for i in range(NSLOT)]
                        qT = [sb(esP, "qT%d" % i, [64, 16, 128], BF16) for i in range(4)]
                        qslot = lambda j: (j % 2) if j < NT else 2 + (j - NT)
                        wq = sb(esP, "wq", [128, 8, 1536], BF16, True)
                        wob = sb(esP, "wob", [128, 8, D], BF16, True)
                        msk = sb(esP, "msk", [128, 2, 512], BF16, True)
                        hbt = sb(esP, "hbt", [128, D], BF16)
                        hT = sb(esP, "hT", [128, 8, 128], BF16)
                        qkv = sb(esP, "qkv", [128, 1536], F32)
                        qkb = sb(esP, "qkb", [128, 20, 64], BF16)
                        rp = [sb(esP, "rp%d" % i, [128, 20, 32], F32) for i in range(2)]
                        cs = sb(esP, "cs", [128, 64], F32)
                        ptl = [sb(esP, "ptl%d" % i, [128, 512], BF16) for i in range(5)]
                        osb = sb(esP, "osb", [128, D], BF16)
                        oT = sb(esP, "oT", [128, 8, 128], BF16)
                        rden = sb(esP, "rden", [128, 4], F32)
                        for i in range(NSLOT):
                            op("dve", lambda e: e.memset(vv[i][:], 1.0), writes=[vv[i]])
                        with ExitStack() as esW:
                            wtmp = sb(esW, "wtmp", [128, 8, 256], F32)
                            for cb in range(6):
                                op("sp", lambda e: e.dma_start(out=wtmp[:], in_=wqkv[jm, :, cb * 256:(cb + 1) * 256].rearrange("(k p) f -> p k f", p=128)), writes=[wtmp], dma=d_m)
                                op("dve", lambda e: e.tensor_copy(out=wq[:, :, cb * 256:(cb + 1) * 256], in_=wtmp[:]), reads=[wtmp], writes=[wq])
                            for cb in range(4):
                                op("sp", lambda e: e.dma_start(out=wtmp[:], in_=wo[jm, :, cb * 256:(cb + 1) * 256].rearrange("(k p) f -> p k f", p=128)), writes=[wtmp], dma=d_m)
                                op("dve", lambda e: e.tensor_copy(out=wob[:, :, cb * 256:(cb + 1) * 256], in_=wtmp[:]), reads=[wtmp], writes=[wob])
                            wm = wtmp[:].rearrange("p k f -> p (k f)")[:, 0:1024].rearrange("p (m f) -> p m f", m=2)
                            op("sp", lambda e: e.dma_start(out=wm, in_=c_masks.rearrange("m p f -> p m f")), writes=[wtmp], dma=d_m)
                            op("dve", lambda e: e.tensor_copy(out=msk[:], in_=wm), reads=[wtmp], writes=[msk])
                            S.barrier()

                    cnt = {"x": 0}

                    def load_x(j):
                        i = cnt["x"] % 2
                        cnt["x"] += 1
                        t = xt[i]
                        op("sp", lambda e: e.dma_start(out=t[:], in_=X[j * 128:(j + 1) * 128, :]), reads=[bX], writes=[t], dma=d_x[i])
                        return t

                    def norm_mod(src, r, A, B, dst):
                        op("act", lambda e: e.activation(out=sqj[:, 0:D], in_=src[:], func=AF.Square, accum_out=st1[:, 0:1]), reads=[src], writes=[sqj, st1])
                        op("dve", lambda e: e.tensor_scalar(out=st1[:, 1:2], in0=st1[:, 0:1], scalar1=1.0 / D, scalar2=EPS, op0=ALU.mult, op1=ALU.add), reads=[st1], writes=[st1])
                        op("act", lambda e: e.activation(out=st1[:, 2:3], in_=st1[:, 1:2], func=AF.Sqrt), reads=[st1], writes=[st1])
                        op("dve", lambda e: e.reciprocal(out=st1[:, 3:4], in_=st1[:, 2:3]), reads=[st1], writes=[st1])
                        op("dve", lambda e: e.scalar_tensor_tensor(out=dst[:], in0=src[:], scalar=st1[:, 3:4], in1=V[r][A][:], op0=ALU.mult, op1=ALU.mult), reads=[src, st1, V[r][A]], writes=[dst])
                        op("pool", lambda e: e.tensor_tensor(out=dst[:], in0=dst[:], in1=V[r][B][:], op=ALU.add), reads=[dst, V[r][B]], writes=[dst])

                    def stageA(j):
                        r = 0 if j < NT else 1
                        s = slot(j)
                        x = load_x(j)
                        norm_mod(x, r, "A1", "B1", hf)
                        if is_pool:
                            op("act", lambda e: e.copy(out=hb[s][:], in_=hf[:]), reads=[hf], writes=[hb[s]])
                            return
                        op("act", lambda e: e.copy(out=hbt[:], in_=hf[:]), reads=[hf], writes=[hbt])
                        for cc in range(8):
                            op("pe", lambda e: e.transpose(out=PT[:, cc * 128:(cc + 1) * 128], in_=hbt[:, cc * 128:(cc + 1) * 128], identity=identb[:]), reads=[hbt, identb], writes=[PT])
                        op("dve", lambda e: e.tensor_copy(out=hT[:].rearrange("p k t -> p (k t)"), in_=PT[:]), reads=[PT], writes=[hT])
                        for cb in range(3):
                            for kc in range(8):
                                op("pe", lambda e: e.matmul(PF[cb][:], lhsT=hT[:, kc, :], rhs=wq[:, kc, cb * 512:(cb + 1) * 512], start=(kc == 0), stop=(kc == 7)), reads=[hT, wq], writes=[PF[cb]])
                            if cb == 1:
                                op("dve", lambda e: e.tensor_copy(out=qkv[:, cb * 512:(cb + 1) * 512], in_=PF[cb][:]), reads=[PF[cb]], writes=[qkv])
                            else:
                                op("act", lambda e: e.copy(out=qkv[:, cb * 512:(cb + 1) * 512], in_=PF[cb][:]), reads=[PF[cb]], writes=[qkv])
                        op("sp", lambda e: e.dma_start(out=cs[:], in_=c_rope[j * 128:(j + 1) * 128, :]), writes=[cs], dma=d_m)
                        op("act", lambda e: e.activation(out=sqj[:, 0:1280], in_=qkv[:, 0:1280], func=AF.Square), reads=[qkv], writes=[sqj])
                        op("dve", lambda e: e.tensor_reduce(out=lg[:, 0:20], in_=sqj[:, 0:1280].rearrange("p (h d) -> p h d", d=64), axis=AX.X, op=ALU.add), reads=[sqj], writes=[lg])
                        op("dve", lambda e: e.tensor_scalar(out=lg[:, 0:20], in0=lg[:, 0:20], scalar1=1.0 / 64, scalar2=EPS, op0=ALU.mult, op1=ALU.add), reads=[lg], writes=[lg])
                        op("act", lambda e: e.activation(out=lg[:, 0:20], in_=lg[:, 0:20], func=AF.Sqrt), reads=[lg], writes=[lg])
                        op("dve", lambda e: e.reciprocal(out=lg[:, 20:40], in_=lg[:, 0:20]), reads=[lg], writes=[lg])
                        q3 = qkv[:, 0:1280].rearrange("p (h d) -> p h d", d=64)
                        op("dve", lambda e: e.tensor_tensor(out=q3, in0=q3, in1=lg[:, 20:40].unsqueeze(2).to_broadcast([128, 20, 64]), op=ALU.mult), reads=[qkv, lg], writes=[qkv])
                        op("pool", lambda e: e.tensor_tensor(out=qkv[:, 0:1280], in0=qkv[:, 0:1280], in1=GQK[:], op=ALU.mult), reads=[qkv, GQK], writes=[qkv])
                        q4 = qkv[:, 0:1280].rearrange("p (h i two) -> p h i two", two=2, i=32)
                        x0 = q4[:, :, :, 0]
                        x1 = q4[:, :, :, 1]
                        o4 = qkb[:].rearrange("p h (i two) -> p h i two", two=2)
                        cosb = cs[:, 0:32].unsqueeze(1).to_broadcast([128, 20, 32])
                        sinb = cs[:, 32:64].unsqueeze(1).to_broadcast([128, 20, 32])
                        op("dve", lambda e: e.tensor_tensor(out=rp[0][:], in0=x0, in1=cosb, op=ALU.mult), reads=[qkv, cs], writes=[rp[0]])
                        op("pool", lambda e: e.tensor_tensor(out=rp[1][:], in0=x1, in1=sinb, op=ALU.mult), reads=[qkv, cs], writes=[rp[1]])
                        op("dve", lambda e: e.tensor_tensor(out=o4[:, :, :, 0], in0=rp[0][:], in1=rp[1][:], op=ALU.subtract), reads=[rp[0], rp[1]], writes=[qkb])
                        op("dve", lambda e: e.tensor_tensor(out=rp[0][:], in0=x0, in1=sinb, op=ALU.mult), reads=[qkv, cs], writes=[rp[0]])
                        op("pool", lambda e: e.tensor_tensor(out=rp[1][:], in0=x1, in1=cosb, op=ALU.mult), reads=[qkv, cs], writes=[rp[1]])
                        op("dve", lambda e: e.tensor_tensor(out=o4[:, :, :, 1], in0=rp[0][:], in1=rp[1][:], op=ALU.add), reads=[rp[0], rp[1]], writes=[qkb])
                        op("act", lambda e: e.copy(out=vv[s][:, :, 0:64], in_=qkv[:, 1280:1536].rearrange("p (h d) -> p h d", d=64)), reads=[qkv], writes=[vv[s]])
                        for b0 in range(0, 20, 8):
                            nh = min(8, 20 - b0)
                            for hh in range(nh):
                                op("pe", lambda e: e.transpose(out=PT[0:64, hh * 128:(hh + 1) * 128], in_=qkb[:, b0 + hh, :], identity=identb[:]), reads=[qkb, identb], writes=[PT])
                            if b0 < 16:
                                op("dve", lambda e: e.tensor_copy(out=qT[qslot(j)][:, b0:b0 + 8, :].rearrange("p h t -> p (h t)"), in_=PT[0:64, 0:1024]), reads=[PT], writes=[qT[qslot(j)]])
                            else:
                                op("act", lambda e: e.copy(out=kT[s][:].rearrange("p h t -> p (h t)"), in_=PT[0:64, 0:512]), reads=[PT], writes=[kT[s]])

                    def mix_pool(j, x):
                        r = 0 if j < NT else 1
                        n_seq = NT if r == 0 else 2
                        j0 = 0 if r == 0 else NT
                        first = (j == j0)
                        lastt = (j == j0 + n_seq - 1)
                        v = 0 if first else (2 if lastt else 1)
                        srcs = []
                        if not first:
                            srcs.append((0, slot(j - 1)))
                        srcs.append((1, slot(j)))
                        if not lastt:
                            srcs.append((2, slot(j + 1)))
                        for cc in range(8):
                            g = cc // 2
                            pb = PF[cc // 4]
                            for si, (rel, ss) in enumerate(srcs):
                                op("pe", lambda e: e.matmul(pb[:, (cc % 4) * 128:(cc % 4 + 1) * 128], lhsT=hb[ss][:, cc * 128:(cc + 1) * 128], rhs=band[:, v * 3 + rel, g, :], start=(si == 0), stop=(si == len(srcs) - 1)), reads=[hb[ss], band], writes=[pb])
                        op("dve", lambda e: e.tensor_copy(out=pT[:, 0:4, :].rearrange("p k t -> p (k t)"), in_=PF[0][:]), reads=[PF[0]], writes=[pT])
                        op("act", lambda e: e.copy(out=pT[:, 4:8, :].rearrange("p k t -> p (k t)"), in_=PF[1][:]), reads=[PF[1]], writes=[pT])
                        for g in range(4):
                            pb = PF[2 + g // 2]
                            for c2 in range(2):
                                op("pe", lambda e: e.matmul(pb[:, (g % 2) * 256:(g % 2 + 1) * 256], lhsT=pT[:, g * 2 + c2, :], rhs=pw[:, g * 2 + c2, :], start=(c2 == 0), stop=(c2 == 1)), reads=[pT, pw], writes=[pb])
                        return [PF[2], PF[3]]

                    def mix_attn(j):
                        r = 0 if j < NT else 1
                        s = slot(j)
                        chunks = []
                        if r == 0:
                            if j > 0:
                                chunks.append((slot(j - 1), 0))
                            chunks.append((s, None))
                            if j < NT - 1:
                                chunks.append((slot(j + 1), 1))
                        chunks.append((3, None))
                        chunks.append((4, None))
                        for hk in range(4):
                            for ci, (ks, mt) in enumerate(chunks):
                                ps_ = PF[3 + ci % 2]
                                op("pe", lambda e: e.matmul(ps_[:], lhsT=kT[ks][:, hk, :], rhs=qT[qslot(j)][:, hk * 4:(hk + 1) * 4, :].rearrange("p h t -> p (h t)"), start=True, stop=True), reads=[kT[ks], qT[qslot(j)]], writes=[ps_])
                                op("act", lambda e: e.activation(out=ptl[ci][:], in_=ps_[:], func=AF.Exp, scale=0.125, bias=SK[:, 0:1]), reads=[ps_, SK], writes=[ptl[ci]])
                                if mt is not None:
                                    op("pool", lambda e: e.tensor_tensor(out=ptl[ci][:], in0=ptl[ci][:], in1=msk[:, mt, :], op=ALU.mult), reads=[ptl[ci], msk], writes=[ptl[ci]])
                            po = PF[5]
                            for hq in range(4):
                                for ci, (ks, mt) in enumerate(chunks):
                                    op("pe", lambda e: e.matmul(po[:, hq * 65:(hq + 1) * 65], lhsT=ptl[ci][:, hq * 128:(hq + 1) * 128], rhs=vv[ks][:, hk, :], start=(ci == 0), stop=(ci == len(chunks) - 1)), reads=[ptl[ci], vv[ks]], writes=[po])
                            po3 = po[:, 0:260].rearrange("p (h d) -> p h d", d=65)
                            op("dve", lambda e: e.tensor_tensor(out=rden[:], in0=po3[:, :, 64], in1=SK[:, 1 + hk * 4:5 + hk * 4], op=ALU.add), reads=[po, SK], writes=[rden])
                            op("dve", lambda e: e.reciprocal(out=rden[:], in_=rden[:]), reads=[rden], writes=[rden])
                            op("dve", lambda e: e.tensor_tensor(out=osb[:, hk * 256:(hk + 1) * 256].rearrange("p (h d) -> p h d", d=64), in0=po3[:, :, 0:64], in1=rden[:].unsqueeze(2).to_broadcast([128, 4, 64]), op=ALU.mult), reads=[po, rden], writes=[osb])
                        for cc in range(8):
                            op("pe", lambda e: e.transpose(out=PT[:, cc * 128:(cc + 1) * 128], in_=osb[:, cc * 128:(cc + 1) * 128], identity=identb[:]), reads=[osb, identb], writes=[PT])
                        op("dve", lambda e: e.tensor_copy(out=oT[:].rearrange("p k t -> p (k t)"), in_=PT[:]), reads=[PT], writes=[oT])
                        for dh in range(2):
                            for cc in range(8):
                                op("pe", lambda e: e.matmul(PF[dh][:], lhsT=oT[:, cc, :], rhs=wob[:, cc, dh * 512:(dh + 1) * 512], start=(cc == 0), stop=(cc == 7)), reads=[oT, wob], writes=[PF[dh]])
                        return [PF[0], PF[1]]

                    cntb = {"b": 0}

                    def stageB(j):
                        r = 0 if j < NT else 1
                        i = cntb["b"] % 2
                        cntb["b"] += 1
                        x = load_x(j)
                        ys = mix_pool(j, x) if is_pool else mix_attn(j)
                        xmi = xm[i]
                        for dh in range(2):
                            op("dve", lambda e: e.tensor_tensor(out=hf[:, dh * 512:(dh + 1) * 512], in0=ys[dh][:], in1=V[r]["G1"][:, dh * 512:(dh + 1) * 512], op=ALU.mult), reads=[ys[dh], V[r]["G1"]], writes=[hf])
                        op("pool", lambda e: e.tensor_tensor(out=xmi[:], in0=hf[:], in1=x[:], op=ALU.add), reads=[hf, x], writes=[xmi])
                        op("sp", lambda e: e.dma_start(out=X[j * 128:(j + 1) * 128, :], in_=xmi[:]), reads=[xmi], writes=[bX], dma=d_o[i])
                        norm_mod(xmi, r, "A2", "B2", h2f)
                        rt = rowt[i]
                        op("act", lambda e: e.copy(out=rt[:, 0:D], in_=h2f[:]), reads=[h2f], writes=[rt])
                        for cc in range(8):
                            pb = PF[5 + cc // 4]
                            op("pe", lambda e: e.transpose(out=pb[:, (cc % 4) * 128:(cc % 4 + 1) * 128], in_=h2f[:, cc * 128:(cc + 1) * 128], identity=identf[:]), reads=[h2f, identf], writes=[pb])
                        op("dve", lambda e: e.tensor_copy(out=h2T[:, 0:4, :].rearrange("p k t -> p (k t)"), in_=PF[5][:]), reads=[PF[5]], writes=[h2T])
                        op("act", lambda e: e.copy(out=h2T[:, 4:8, :].rearrange("p k t -> p (k t)"), in_=PF[6][:]), reads=[PF[6]], writes=[h2T])
                        for cc in range(8):
                            op("pe", lambda e: e.matmul(PF[6][:, 0:NE], lhsT=h2T[:, cc, :], rhs=wr[:, cc, :], start=(cc == 0), stop=(cc == 7)), reads=[h2T, wr], writes=[PF[6]])
                        op("dve", lambda e: e.tensor_reduce(out=lg[:, 40:41], in_=PF[6][:, 0:NE], axis=AX.X, op=ALU.max), reads=[PF[6]], writes=[lg])
                        op("dve", lambda e: e.tensor_scalar(out=lg[:, 41:42], in0=lg[:, 40:41], scalar1=-1.0, scalar2=None, op0=ALU.mult), reads=[lg], writes=[lg])
                        op("act", lambda e: e.activation(out=lg[:, 44:60], in_=PF[6][:, 0:NE], func=AF.Exp, bias=lg[:, 41:42], accum_out=lg[:, 42:43]), reads=[PF[6], lg], writes=[lg])
                        op("dve", lambda e: e.reciprocal(out=lg[:, 43:44], in_=lg[:, 42:43]), reads=[lg], writes=[lg])
                        op("dve", lambda e: e.tensor_scalar(out=AFF[:, j, :], in0=lg[:, 44:60], scalar1=lg[:, 43:44], scalar2=None, op0=ALU.mult), reads=[lg], writes=[AFF])
                        auxf = rt[:, D:RW].bitcast(F32)
                        auxi = rt[:, D:RW].bitcast(I32)
                        op("dve", lambda e: e.tensor_copy(out=auxf[:, 1:17], in_=AFF[:, j, :]), reads=[AFF], writes=[rt])
                        op("dve", lambda e: e.tensor_copy(out=auxi[:, 0:1], in_=tokid[:, j:j + 1]), reads=[tokid], writes=[rt])
                        op("sp", lambda e: e.dma_start(out=H2[j * 128:(j + 1) * 128, :], in_=rt[:]), reads=[rt], writes=[bH2], dma=d_r[i])

                    for i in range(2):
                        op("dve", lambda e: e.memset(rowt[i][:], 0.0), writes=[rowt[i]])
                    stageA(NT); stageA(NT + 1)
                    if upd_ctx:
                        stageB(NT); stageB(NT + 1)
                    stageA(0)
                    for j in range(NT):
                        if j + 1 < NT:
                            stageA(j + 1)
                        stageB(j)
                    S.barrier()
                    esP.close()

                    with ExitStack() as esR:
                        NJ = [NT, 2]
                        J0 = [0, NT]
                        lo = sb(esR, "lo", [128, 2, NE], F32); hi = sb(esR, "hi", [128, 2, NE], F32); mid = sb(esR, "mid", [128, 2, NE], F32)
                        cvc = sb(esR, "cvc", [128, 2, NE], F32)
                        cmpb = sb(esR, "cmpb", [128, NTT, NE], BF16)
                        part = sb(esR, "part", [128, 2, NE], BF16)
                        fl = sb(esR, "fl", [128, 2, NE], U32); nfl = sb(esR, "nfl", [128, 2, NE], U32)
                        op("dve", lambda e: e.memset(lo[:], 0.0), writes=[lo])
                        op("dve", lambda e: e.memset(hi[:], 1.0), writes=[hi])
                        op("dve", lambda e: e.memset(mid[:], 0.5), writes=[mid])
                        op("dve", lambda e: e.memset(cvc[:, 0, :], float(CL)), writes=[cvc])
                        op("dve", lambda e: e.memset(cvc[:, 1, :], 32.0), writes=[cvc])

                        def compare(th):
                            for s_ in range(2):
                                if s_ == 1 and not upd_ctx:
                                    op("dve", lambda e: e.memset(cmpb[:, NT:NTT, :], 0.0), writes=[cmpb])
                                    continue
                                op("dve", lambda e: e.tensor_tensor(out=cmpb[:, J0[s_]:J0[s_] + NJ[s_], :], in0=AFF[:, J0[s_]:J0[s_] + NJ[s_], :], in1=th[:, s_, :].unsqueeze(1).to_broadcast([128, NJ[s_], NE]), op=ALU.is_ge), reads=[AFF, th], writes=[cmpb])

                        for it in range(34):
                            compare(mid)
                            for s_ in range(2):
                                op("dve", lambda e: e.tensor_reduce(out=part[:, s_, :], in_=cmpb[:, J0[s_]:J0[s_] + NJ[s_], :].rearrange("p j e -> p e j"), axis=AX.X, op=ALU.add), reads=[cmpb], writes=[part])
                            op("pe", lambda e: e.matmul(PF[0][:, 0:32], lhsT=onesb[:], rhs=part[:].rearrange("p s e -> p (s e)"), start=True, stop=True), reads=[part, onesb], writes=[PF[0]])
                            op("dve", lambda e: e.tensor_tensor(out=fl[:].rearrange("p s e -> p (s e)"), in0=PF[0][:, 0:32], in1=cvc[:].rearrange("p s e -> p (s e)"), op=ALU.is_ge), reads=[PF[0], cvc], writes=[fl])
                            op("dve", lambda e: e.tensor_tensor(out=nfl[:].rearrange("p s e -> p (s e)"), in0=PF[0][:, 0:32], in1=cvc[:].rearrange("p s e -> p (s e)"), op=ALU.is_lt), reads=[PF[0], cvc], writes=[nfl])
                            op("dve", lambda e: e.copy_predicated(out=lo[:], mask=fl[:], data=mid[:]), reads=[fl, mid, lo], writes=[lo])
                            op("dve", lambda e: e.copy_predicated(out=hi[:], mask=nfl[:], data=mid[:]), reads=[nfl, mid, hi], writes=[hi])
                            op("dve", lambda e: e.tensor_tensor(out=mid[:], in0=lo[:], in1=hi[:], op=ALU.add), reads=[lo, hi], writes=[mid])
                            op("dve", lambda e: e.tensor_scalar(out=mid[:], in0=mid[:], scalar1=0.5, scalar2=None, op0=ALU.mult), reads=[mid], writes=[mid])
                        compare(lo)
                        M2 = sb(esR, "M2", [128, NTT, NE], F32)
                        CSa = sb(esR, "CSa", [128, NTT, NE], F32)
                        CSb = sb(esR, "CSb", [128, NTT, NE], F32)
                        CS0 = sb(esR, "CS0", [128, NTT, NE], F32)
                        ncol = NTT * NE
                        cm2 = cmpb[:].rearrange("p j e -> p (j e)")
                        for (dst, lt) in ((M2, lstr), (CS0, onesb)):
                            d2 = dst[:].rearrange("p j e -> p (j e)")
                            for c0 in range(0, ncol, 512):
                                n = min(512, ncol - c0)
                                pb = PF[(c0 // 512) % 2]
                                op("pe", lambda e: e.matmul(pb[:, 0:n], lhsT=lt[:], rhs=cm2[:, c0:c0 + n], start=True, stop=True), reads=[cmpb, lt], writes=[pb])
                                op("dve", lambda e: e.tensor_copy(out=d2[:, c0:c0 + n], in_=pb[:, 0:n]), reads=[pb], writes=[dst])
                        op("dve", lambda e: e.tensor_copy(out=CSa[:], in_=CS0[:]), reads=[CS0], writes=[CSa])
                        a, b = CSa, CSb
                        dd = 1
                        while dd < NT:
                            op("dve", lambda e: e.tensor_tensor(out=b[:, dd:NT, :], in0=a[:, dd:NT, :], in1=a[:, 0:NT - dd, :], op=ALU.add), reads=[a], writes=[b])
                            op("pool", lambda e: e.tensor_copy(out=b[:, 0:dd, :], in_=a[:, 0:dd, :]), reads=[a], writes=[b])
                            a, b = b, a
                            dd *= 2
                        op("dve", lambda e: e.tensor_tensor(out=a[:, 0:NT, :], in0=a[:, 0:NT, :], in1=CS0[:, 0:NT, :], op=ALU.subtract), reads=[a, CS0], writes=[a])
                        op("dve", lambda e: e.tensor_tensor(out=M2[:, 0:NT, :], in0=M2[:, 0:NT, :], in1=a[:, 0:NT, :], op=ALU.add), reads=[a, M2], writes=[M2])
                        op("dve", lambda e: e.tensor_tensor(out=M2[:, NT + 1, :], in0=M2[:, NT + 1, :], in1=CS0[:, NT, :], op=ALU.add), reads=[M2, CS0], writes=[M2])
                        op("dve", lambda e: e.tensor_scalar(out=M2[:, NT:NTT, :], in0=M2[:, NT:NTT, :], scalar1=float(CL), scalar2=None, op0=ALU.add), reads=[M2], writes=[M2])
                        op("dve", lambda e: e.tensor_scalar(out=CS0[:], in0=cmpb[:], scalar1=-BIG, scalar2=BIG, op0=ALU.mult, op1=ALU.add), reads=[cmpb], writes=[CS0])
                        op("dve", lambda e: e.tensor_tensor(out=M2[:], in0=M2[:], in1=CS0[:], op=ALU.add), reads=[M2, CS0], writes=[M2])
                        op("dve", lambda e: e.tensor_copy(out=POSI[:], in_=M2[:]), reads=[M2], writes=[POSI])
                        S.barrier()
                esT.close()
                S.barrier()

                with ExitStack() as esE:
                    d_rl = [S.dsem("d_rl%d" % i) for i in range(4)]
                    d_sc = [S.dsem("d_sc%d" % i) for i in range(4)]
                    d_xl = [S.dsem("d_xl%d" % i) for i in range(2)]
                    d_ws = [S.dsem("d_ws%d" % i) for i in range(3)]
                    d_ya = [S.dsem("d_ya%d" % i) for i in range(2)]
                    rring = [sb(esE, "rring%d" % i, [128, RW], BF16) for i in range(3)]
                    xsl = [sb(esE, "xsl%d" % i, [128, RW], BF16) for i in range(2)]
                    xsT = sb(esE, "xsT", [128, 8, NSL], BF16)
                    hTa = sb(esE, "hTa", [128, 16, NSL], BF16)
                    WGs = [sb(esE, "WGs%d" % i, [128, 8, 256], BF16) for i in range(2)]
                    WUs = [sb(esE, "WUs%d" % i, [128, 8, 256], BF16) for i in range(2)]
                    WD = [sb(esE, "WD%d" % i, [128, 2, D], BF16) for i in range(8)]
                    wstg = [sb(esE, "wstg%d" % i, [128, 2048], F32) for i in range(2)]
                    ysb = [sb(esE, "ysb%d" % i, [128, D], F32) for i in range(2)]
                    sact = [sb(esE, "sact0", [128, 512], F32)] * 2
                    IDX = [sb(esE, "IDX%d" % i, [128, NS], I32) for i in range(2)]
                    VAL = [sb(esE, "VAL%d" % i, [128, NS], F32) for i in range(2)]
                    tiles = list(range(NT)) + ([NT, NT + 1] if upd_ctx else [])
                    cw = {"s": 0, "r": 0, "y": 0, "p": 0}
                    gscr = sb(esE, "gscr", [128, 4], F32)

                    def gate(buf):
                        op("pool", lambda e: e.memset(gscr[:, 0:1], 0.0), writes=[buf, gscr])

                    def compaction(e_):
                        if e_ >= NE:
                            return
                        DEPTH = 2
                        pend = []

                        def scat(j, rr, i):
                            bnd = reg_lat if j < NT else reg_ctx
                            op("pool", lambda e: e.indirect_dma_start(out=XS[e_ % 3][:, :], out_offset=bass.IndirectOffsetOnAxis(ap=POSI[:, j, e_:e_ + 1], axis=0), in_=rr[:], in_offset=None, bounds_check=bnd, oob_is_err=False), reads=[rr, POSI, bXS[e_ % 3]], writes=[], dma=d_sc[i])

                        gate(bXS[e_ % 3])
                        for j in tiles:
                            i = cw["r"] % 3
                            cw["r"] += 1
                            rr = rring[i]
                            op("pool", lambda e: e.dma_start(out=rr[:], in_=H2[j * 128:(j + 1) * 128, :]), reads=[bH2], writes=[rr], dma=d_rl[i])
                            pend.append((j, rr, i))
                            if len(pend) > DEPTH:
                                scat(*pend.pop(0))
                            yield
                        while pend:
                            scat(*pend.pop(0))

                    def gu_dma(gp):
                        e2, pc = gp // 8, gp % 8
                        if e2 >= NE:
                            return
                        sg = wstg[0][:].rearrange("p (a b) -> p a b", a=8)
                        su = wstg[1][:].rearrange("p (a b) -> p a b", a=8)
                        op("sp", lambda e: e.dma_start(out=sg, in_=wg_in[l, e2, :, pc * 256:(pc + 1) * 256].rearrange("(k p) f -> p k f", p=128)), writes=[wstg[0]], dma=d_ws[0])
                        op("sp", lambda e: e.dma_start(out=su, in_=wu_in[l, e2, :, pc * 256:(pc + 1) * 256].rearrange("(k p) f -> p k f", p=128)), writes=[wstg[1]], dma=d_ws[1])

                    def gu_cast(gp):
                        e2 = gp // 8
                        if e2 >= NE:
                            return
                        sg = wstg[0][:].rearrange("p (a b) -> p a b", a=8)
                        su = wstg[1][:].rearrange("p (a b) -> p a b", a=8)
                        op("act", lambda e: e.copy(out=WGs[gp % 2][:], in_=sg), reads=[wstg[0]], writes=[WGs[gp % 2]])
                        op("dve", lambda e: e.tensor_copy(out=WUs[gp % 2][:], in_=su), reads=[wstg[1]], writes=[WUs[gp % 2]])

                    def wd_dma(gp):
                        e2, pc = gp // 8, gp % 8
                        if e2 >= NE:
                            return
                        op("pool", lambda e: e.dma_start(out=WD[pc][:], in_=wd_in[l, e2, pc * 256:(pc + 1) * 256, :].rearrange("(c p) d -> p c d", p=128)), writes=[WD[pc]], dma=d_ws[2])

                    def E2(e_):
                        par = e_ % 2
                        gate(bXS[e_ % 3])
                        for st_i in range(NS):
                            xl = xsl[st_i % 2]
                            op("sp", lambda e: e.dma_start(out=xl[:], in_=XS[e_ % 3][st_i * 128:(st_i + 1) * 128, :]), reads=[bXS[e_ % 3]], writes=[xl], dma=d_xl[st_i % 2])
                            op("pool", lambda e: e.tensor_copy(out=IDX[par][:, st_i:st_i + 1], in_=xl[:, D:RW].bitcast(I32)[:, 0:1]), reads=[xl], writes=[IDX[par]])
                            op("pool", lambda e: e.tensor_copy(out=VAL[par][:, st_i:st_i + 1], in_=xl[:, D:RW].bitcast(F32)[:, 1 + e_:2 + e_]), reads=[xl], writes=[VAL[par]])
                            for cc in range(8):
                                op("pe", lambda e: e.transpose(out=PT[:, cc * 128:(cc + 1) * 128], in_=xl[:, cc * 128:(cc + 1) * 128], identity=identb[:]), reads=[xl, identb], writes=[PT])
                            op("dve", lambda e: e.tensor_copy(out=xsT[:, :, st_i * 128:(st_i + 1) * 128], in_=PT[:].rearrange("p (k t) -> p k t", t=128)), reads=[PT], writes=[xsT])

                    for ee in range(2):
                        for _ in compaction(ee):
                            pass
                    gu_dma(0)
                    gu_cast(0)
                    gu_dma(1)
                    E2(0)
                    for e_ in range(NE):
                        par = e_ % 2
                        gen = compaction(e_ + 2)
                        for pc in range(8):
                            gp = e_ * 8 + pc
                            wg_t = WGs[gp % 2]; wu_t = WUs[gp % 2]
                            it_ = 0
                            n_it = len(TT) * 2
                            for ti, (s0_, n) in enumerate(TT):
                                for f2 in range(2):
                                    if it_ == max(n_it - 3, 0):
                                        gu_cast(gp + 1)
                                        gu_dma(gp + 2)
                                        wd_dma(gp)
                                    it_ += 1
                                    fc = pc * 2 + f2
                                    k_ = cw["p"] % 2
                                    cw["p"] += 1
                                    pa = PF[k_ * 2]; pu = PF[k_ * 2 + 1]
                                    for kc in range(8):
                                        op("pe", lambda e: e.matmul(pa[:, 0:n], lhsT=wg_t[:, kc, f2 * 128:(f2 + 1) * 128], rhs=xsT[:, kc, s0_:s0_ + n], start=(kc == 0), stop=(kc == 7)), reads=[wg_t, xsT], writes=[pa])
                                    for kc in range(8):
                                        op("pe", lambda e: e.matmul(pu[:, 0:n], lhsT=wu_t[:, kc, f2 * 128:(f2 + 1) * 128], rhs=xsT[:, kc, s0_:s0_ + n], start=(kc == 0), stop=(kc == 7)), reads=[wu_t, xsT], writes=[pu])
                                    sa = sact[k_]
                                    op("act", lambda e: e.activation(out=sa[:, 0:n], in_=pa[:, 0:n], func=AF.Silu), reads=[pa], writes=[sa])
                                    op("dve", lambda e: e.tensor_tensor(out=hTa[:, fc, s0_:s0_ + n], in0=sa[:, 0:n], in1=pu[:, 0:n], op=ALU.mult), reads=[sa, pu], writes=[hTa])
                                    next(gen, None)
                                    next(gen, None)
                        for _ in gen:
                            pass
                        if e_ + 1 < NE:
                            E2(e_ + 1)
                        gate(bX)
                        for st_i in range(NS):
                            is_ctx = (st_i == NS - 1)
                            if is_ctx and not upd_ctx:
                                continue
                            yi = cw["y"] % 2
                            yb = ysb[yi]
                            cw["y"] += 1
                            g2 = G2[1] if is_ctx else G2[0]
                            for dh in range(2):
                                py = PF[4 + dh]
                                for fc in range(16):
                                    op("pe", lambda e: e.matmul(py[:], lhsT=hTa[:, fc, st_i * 128:(st_i + 1) * 128], rhs=WD[fc // 2][:, fc % 2, dh * 512:(dh + 1) * 512], start=(fc == 0), stop=(fc == 15)), reads=[hTa, WD[fc // 2]], writes=[py])
                                op("dve", lambda e: e.scalar_tensor_tensor(out=yb[:, dh * 512:(dh + 1) * 512], in0=py[:], scalar=VAL[par][:, st_i:st_i + 1], in1=g2[:, dh * 512:(dh + 1) * 512], op0=ALU.mult, op1=ALU.mult), reads=[py, VAL[par], g2], writes=[yb])
                            npart = 32 if is_ctx else 128
                            op("pool", lambda e: e.indirect_dma_start(out=X[:, :], out_offset=bass.IndirectOffsetOnAxis(ap=IDX[par][0:npart, st_i:st_i + 1], axis=0), in_=yb[0:npart, :], in_offset=None, compute_op=ALU.add), reads=[yb, IDX[par], bX], writes=[], dma=d_ya[yi])
                    S.barrier()
        d_out = S.dsem("d_out")
        CH = 1024
        for r0 in range(0, L, CH):
            n = min(CH, L - r0)
            op("sp", lambda e: e.dma_start(out=out_d[r0:r0 + n, :], in_=X[r0:r0 + n, :]), reads=[bX], writes=[bOut], dma=d_out)
        S._wait("sp", [bOut.last_w])
    return nc


_CACHE = {}


def kernel(x, c, ctx, c_ctx, w_ada, b_ada, norm1_g, norm2_g, pool_w, pool_scale,
           attn_w_qkv, attn_w_o, attn_q_norm, attn_k_norm, attn_sink,
           router_w, exp_w_gate, exp_w_up, exp_w_down):
    def f(a):
        a = np.ascontiguousarray(np.asarray(a), dtype=np.float32)
        if a.shape[0] == 0:
            a = np.zeros((1,) + a.shape[1:], np.float32)
        return a
    x = f(x); c = f(c); ctx = f(ctx); c_ctx = f(c_ctx)
    B, L, _ = x.shape
    depth = w_ada.shape[0]
    n_pool = pool_w.shape[0]; n_attn = attn_w_qkv.shape[0]
    NT = L // 128; NTT = NT + 2
    key = (L, depth)
    if key not in _CACHE:
        _CACHE[key] = build(L, depth, n_pool, n_attn)
    nc = _CACHE[key]
    ident = np.eye(128, dtype=np.float32)
    lstrict = (np.arange(128)[:, None] < np.arange(128)[None, :]).astype(np.float32)
    kk = np.arange(128)[:, None]; qq = np.arange(128)[None, :]
    m0 = np.tile((kk >= qq).astype(np.float32), (1, 4)); m1 = np.tile((kk <= qq).astype(np.float32), (1, 4))
    masks = np.stack([m0, m1], 0)
    band = band_consts()
    tokid = (np.arange(NTT)[None, :] * 128 + np.arange(128)[:, None]).astype(np.int32)
    pos = np.arange(L)
    inv = (10000.0 ** (-np.arange(16, dtype=np.float32) / 16)).astype(np.float32)
    ang = np.concatenate([(pos // 64).astype(np.float32)[:, None] * inv, (pos % 64).astype(np.float32)[:, None] * inv], -1).astype(np.float32)
    rope = np.zeros((NTT * 128, 64), np.float32)
    rope[:L, :32] = np.cos(ang); rope[:L, 32:] = np.sin(ang)
    rope[L:, :32] = 1.0
    gqk = np.concatenate([np.tile(f(attn_q_norm), (1, 16)), np.tile(f(attn_k_norm), (1, 4))], 1)
    shared = {
        "w_ada": f(w_ada), "b_ada": f(b_ada), "norm1_g": f(norm1_g), "norm2_g": f(norm2_g),
        "pool_w": f(pool_w), "pool_scale": f(pool_scale), "attn_w_qkv": f(attn_w_qkv), "attn_w_o": f(attn_w_o),
        "gqk": gqk, "attn_q_norm": f(attn_q_norm), "attn_k_norm": f(attn_k_norm), "attn_sink": f(attn_sink),
        "router_w": f(router_w), "exp_w_gate": f(exp_w_gate), "exp_w_up": f(exp_w_up), "exp_w_down": f(exp_w_down),
        "c_ident": ident, "c_lstrict": lstrict, "c_masks": masks, "c_band": band, "c_tokid": tokid, "c_rope": rope,
    }
    in_maps = []
    for k in range(8):
        b = (k * B) // 8
        cv = np.stack([c[b], c_ctx], -1)
        cvT = np.ascontiguousarray(cv.reshape(8, 128, 2).transpose(1, 0, 2))
        m = dict(shared)
        m.update({"x": x[b], "ctx": ctx[b], "cvT": cvT})
        in_maps.append(m)
    res = run_bass_kernel_spmd(nc, in_maps, core_ids=list(range(8)))
    out = np.stack([np.asarray(res.results[(b * 8) // B]["out"]) for b in range(B)], 0)
    return out.astype(np.float32)
```

```python
import numpy as np
from contextlib import ExitStack
import concourse.bass as bass
import concourse.mybir as mybir
from concourse.bass_utils import run_bass_kernel_spmd

F32 = mybir.dt.float32
BF16 = mybir.dt.bfloat16
I32 = mybir.dt.int32
U32 = mybir.dt.uint32
AF = mybir.ActivationFunctionType
ALU = mybir.AluOpType
AX = mybir.AxisListType

D = 1024
CTX = 256
NE = 16
DE = 2048
RW = 1088
EPS = 1e-6
POOL_W = (2, 4, 8, 16)
BIG = 1.0e6


class Sem:
    def __init__(self, nc, name):
        self.h = nc.alloc_semaphore(name)
        self.count = 0


class Buf:
    def __init__(self, name, const=False):
        self.name = name
        self.last_w = None
        self.readers = []
        self.const = const


class Tl:
    def __init__(self, t, name, const=False):
        self.t = t
        self.b = Buf(name, const)

    def __getitem__(self, k):
        return self.t[k]


def _b(x):
    return x.b if isinstance(x, Tl) else x


class Sched:
    def __init__(self, nc):
        self.nc = nc
        self.eng = {"pe": nc.tensor, "act": nc.scalar, "dve": nc.vector, "pool": nc.gpsimd, "sp": nc.sync}
        self.esem = {k: Sem(nc, "es_" + k) for k in ["pe", "act", "dve", "pool"]}
        self.seen = {k: {} for k in self.eng}
        self.dsems = []
        self.named = {}
        self.nops = 0

    def dsem(self, name):
        base = name.split("@")[0]
        if base in self.named:
            return self.named[base]
        s = Sem(self.nc, base)
        self.dsems.append(s)
        self.named[base] = s
        return s

    def _wait(self, engname, deps):
        best = {}
        for s, v in deps:
            if best.get(s, 0) < v:
                best[s] = v
        for s, v in best.items():
            if self.seen[engname].get(s, 0) >= v:
                continue
            self.eng[engname].wait_ge(s.h, v)
            self.seen[engname][s] = v

    def op(self, engname, fn, reads=(), writes=(), dma=None):
        deps = []
        for b in reads:
            b = _b(b)
            if b.last_w is not None:
                deps.append(b.last_w)
        for b in writes:
            b = _b(b)
            if b.last_w is not None:
                deps.append(b.last_w)
            deps.extend(b.readers)
        if engname == "pe":
            deps = [d for d in deps if d[0] is not self.esem["pe"]]
        self._wait(engname, deps)
        ins = fn(self.eng[engname])
        self.nops += 1
        if dma is not None:
            dma.count += 16
            ins.then_inc(dma.h, 16)
            tok = (dma, dma.count)
        else:
            s = self.esem[engname]
            s.count += 1
            ins.then_inc(s.h, 1)
            tok = (s, s.count)
        for b in reads:
            b = _b(b)
            if not b.const:
                b.readers.append(tok)
        for b in writes:
            b = _b(b)
            b.last_w = tok
            b.readers = []
        return tok

    def barrier(self):
        toks = [(s, s.count) for s in self.esem.values() if s.count > 0]
        toks += [(d, d.count) for d in self.dsems if d.count > 0]
        for e in self.eng:
            self._wait(e, toks)


def band_consts():
    out = np.zeros((3, 3, 128, 4, 128), np.float32)
    for v in range(3):
        for g, w in enumerate(POOL_W):
            half = w // 2
            for t in range(128):
                lo, hi = t - half, t + half
                lo_c = max(lo, 0) if v == 0 else lo
                hi_c = min(hi, 128) if v == 2 else hi
                cnt = hi_c - lo_c
                for tp in range(lo_c, hi_c):
                    if tp < 0:
                        out[v, 0, 128 + tp, g, t] += 1.0 / cnt
                    elif tp >= 128:
                        out[v, 2, tp - 128, g, t] += 1.0 / cnt
                    else:
                        out[v, 1, tp, g, t] += 1.0 / cnt
                out[v, 1, t, g, t] -= 1.0
    return out


def build(L, depth, n_pool, n_attn):
    NT = L // 128
    NTT = NT + 2
    CL = L // 8
    NS = CL // 128 + 1
    NSL = CL + 128
    TT = []
    s0 = 0
    while s0 < NSL:
        n = min(512, NSL - s0)
        TT.append((s0, n))
        s0 += n
    nc = bass.Bass("TRN2", target_bir_lowering=False)

    def din(name, shape, dt=F32):
        return nc.dram_tensor(name, list(shape), dt, kind="ExternalInput").ap()

    x_in = din("x", [L, D]); ctx_in = din("ctx", [CTX, D]); cvT = din("cvT", [128, 8, 2])
    w_ada = din("w_ada", [depth, D, 6 * D]); b_ada = din("b_ada", [depth, 6 * D])
    n1g = din("norm1_g", [depth, D]); n2g = din("norm2_g", [depth, D])
    pool_w = din("pool_w", [max(n_pool, 1), 4, 256, 256]); pool_sc = din("pool_scale", [max(n_pool, 1), D])
    wqkv = din("attn_w_qkv", [max(n_attn, 1), D, 1536]); wo = din("attn_w_o", [max(n_attn, 1), D, D])
    gqk_in = din("gqk", [max(n_attn, 1), 1280]); gq_in = din("attn_q_norm", [max(n_attn, 1), 64]); gk_in = din("attn_k_norm", [max(n_attn, 1), 64])
    sink_in = din("attn_sink", [max(n_attn, 1), 16])
    wr_in = din("router_w", [depth, D, NE])
    wg_in = din("exp_w_gate", [depth, NE, D, DE]); wu_in = din("exp_w_up", [depth, NE, D, DE]); wd_in = din("exp_w_down", [depth, NE, DE, D])
    c_ident = din("c_ident", [128, 128]); c_lstrict = din("c_lstrict", [128, 128]); c_masks = din("c_masks", [2, 128, 512])
    c_band = din("c_band", [3, 3, 128, 4, 128]); c_tokid = din("c_tokid", [128, NTT], I32); c_rope = din("c_rope", [NTT * 128, 64])
    out_d = nc.dram_tensor("out", [L, D], F32, kind="ExternalOutput").ap()
    X = nc.dram_tensor("Xres", [NTT * 128, D], F32, kind="Internal").ap()
    H2 = nc.dram_tensor("H2rows", [NTT * 128, RW], BF16, kind="Internal").ap()
    XS = [nc.dram_tensor("XS%d" % i, [NSL, RW], BF16, kind="Internal").ap() for i in range(3)]
    bX = Buf("X"); bH2 = Buf("H2"); bXS = [Buf("XS0"), Buf("XS1"), Buf("XS2")]; bOut = Buf("out")

    S = Sched(nc)
    op = S.op
    with ExitStack() as top:
        uid = [0]

        def sb(es, name, shape, dt, const=False):
            uid[0] += 1
            name = "%s_u%d" % (name, uid[0])
            return Tl(es.enter_context(nc.sbuf_tensor(name, list(shape), dt)), name, const)

        top.enter_context(nc.allow_low_precision(reason="bf16 0/1 masks and small integer counts are exact; bf16 matmul operands by design"))
        PF = [Tl(top.enter_context(nc.psum_tensor("pf%d" % i, [128, 512], F32)), "pf%d" % i) for i in range(7)]
        PT = Tl(top.enter_context(nc.psum_tensor("ptb", [128, 1024], BF16)), "ptb")
        d_c = S.dsem("d_c")
        reg_lat = nc.gpsimd.alloc_register("bnd_lat"); nc.gpsimd.reg_mov(reg_lat, CL - 1)
        reg_ctx = nc.gpsimd.alloc_register("bnd_ctx"); nc.gpsimd.reg_mov(reg_ctx, CL + 31)
        identf = sb(top, "identf", [128, 128], F32, True)
        identb = sb(top, "identb", [128, 128], BF16, True)
        lstr = sb(top, "lstr", [128, 128], BF16, True)
        onesb = sb(top, "onesb", [128, 128], BF16, True)
        ones1 = sb(top, "ones1", [1, 128], F32, True)
        tokid = sb(top, "tokid", [128, NTT], I32, True)
        cvec = sb(top, "cvec", [128, 8, 2], F32, True)
        scT = sb(top, "scT", [128, 8, 2], F32, True)
        G2 = [sb(top, "G2_%d" % r, [128, D], F32) for r in range(2)]
        POSI = sb(top, "POSI", [128, NTT, NE], I32)
        with ExitStack() as es0:
            stg = sb(es0, "cstg", [128, 128], F32)
            zt = sb(es0, "zt", [128, RW], BF16)
            op("sp", lambda e: e.dma_start(out=identf[:], in_=c_ident[:, :]), writes=[identf], dma=d_c)
            op("sp", lambda e: e.dma_start(out=stg[:], in_=c_lstrict[:, :]), writes=[stg], dma=d_c)
            op("sp", lambda e: e.dma_start(out=tokid[:], in_=c_tokid[:, :]), writes=[tokid], dma=d_c)
            op("sp", lambda e: e.dma_start(out=cvec[:], in_=cvT[:, :, :]), writes=[cvec], dma=d_c)
            S.barrier()
            op("dve", lambda e: e.tensor_copy(out=identb[:], in_=identf[:]), reads=[identf], writes=[identb])
            op("dve", lambda e: e.tensor_copy(out=lstr[:], in_=stg[:]), reads=[stg], writes=[lstr])
            op("dve", lambda e: e.memset(onesb[:], 1.0), writes=[onesb])
            op("dve", lambda e: e.memset(ones1[:], 1.0), writes=[ones1])
            op("act", lambda e: e.activation(out=scT[:], in_=cvec[:], func=AF.Silu), reads=[cvec], writes=[scT])
            op("dve", lambda e: e.memset(zt[:], 0.0), writes=[zt])
            for i in range(3):
                for r0 in range(0, NSL, 128):
                    op("sp", lambda e: e.dma_start(out=XS[i][r0:r0 + 128, :], in_=zt[:]), reads=[zt], writes=[bXS[i]], dma=d_c)
            CH = 1024
            for r0 in range(0, L, CH):
                n = min(CH, L - r0)
                op("sp", lambda e: e.dma_start(out=X[r0:r0 + n, :], in_=x_in[r0:r0 + n, :]), writes=[bX], dma=d_c)
            op("sp", lambda e: e.dma_start(out=X[L:L + CTX, :], in_=ctx_in[:, :]), writes=[bX], dma=d_c)
            S.barrier()

        for l in range(depth):
            is_pool = (l % 2 == 0)
            jm = l // 2
            last = (l == depth - 1)
            upd_ctx = not last
            with ExitStack() as esL:
                V = [dict(), dict()]
                names = ["A1", "B1", "G1", "A2", "B2"]
                esT = ExitStack()
                esL.enter_context(esT)
                for r in range(2):
                    for nm in names:
                        V[r][nm] = sb(esT, "V%d%s" % (r, nm), [128, D], F32)
                    V[r]["G2"] = G2[r]
                AFF = sb(esT, "AFF", [128, NTT, NE], F32)
                if not is_pool:
                    GQK = sb(esT, "GQK", [128, 1280], F32)
                    SK = sb(esT, "SK", [128, 17], F32)
                with ExitStack() as esA:
                    d_w = [S.dsem("d_adaw%d" % i) for i in range(2)]
                    d_s = S.dsem("d_adas")
                    wst = [sb(esA, "adaw%d" % i, [128, 8, 512], F32) for i in range(2)]
                    mrow = [sb(esA, "mrow%d" % r, [1, 6 * D], F32) for r in range(2)]
                    brow = sb(esA, "brow", [1, 6 * D], F32)
                    g1row = sb(esA, "g1row", [1, D], F32); g2row = sb(esA, "g2row", [1, D], F32); psrow = sb(esA, "psrow", [1, D], F32)
                    vrow = sb(esA, "vrow", [1, D], F32)
                    op("sp", lambda e: e.dma_start(out=brow[:], in_=b_ada[l:l + 1, :]), writes=[brow], dma=d_s)
                    op("sp", lambda e: e.dma_start(out=g1row[:], in_=n1g[l:l + 1, :]), writes=[g1row], dma=d_s)
                    op("sp", lambda e: e.dma_start(out=g2row[:], in_=n2g[l:l + 1, :]), writes=[g2row], dma=d_s)
                    if is_pool:
                        op("sp", lambda e: e.dma_start(out=psrow[:], in_=pool_sc[jm:jm + 1, :]), writes=[psrow], dma=d_s)
                    if not is_pool:
                        qrow = sb(esA, "qrow", [1, 1280], F32)
                        grow = sb(esA, "grow", [1, 128], F32)
                        srow = sb(esA, "srow", [1, 32], F32)
                        op("sp", lambda e: e.dma_start(out=qrow[:], in_=gqk_in[jm:jm + 1, :]), writes=[qrow], dma=d_s)
                        op("sp", lambda e: e.dma_start(out=grow[0:1, 0:64], in_=gq_in[jm:jm + 1, :]), writes=[grow], dma=d_s)
                        op("sp", lambda e: e.dma_start(out=grow[0:1, 64:128], in_=gk_in[jm:jm + 1, :]), writes=[grow], dma=d_s)
                        op("sp", lambda e: e.dma_start(out=srow[0:1, 1:17], in_=sink_in[jm:jm + 1, :]), writes=[srow], dma=d_s)
                    S.barrier()
                    for nb in range(12):
                        w = wst[nb % 2]
                        op("sp", lambda e: e.dma_start(out=w[:], in_=w_ada[l, :, nb * 512:(nb + 1) * 512].rearrange("(k p) f -> p k f", p=128)), writes=[w], dma=d_w[nb % 2])
                        for r in range(2):
                            pm = PF[r]
                            for kc in range(8):
                                op("pe", lambda e: e.matmul(pm[0:1, :], lhsT=scT[:, kc, r:r + 1], rhs=w[:, kc, :], start=(kc == 0), stop=(kc == 7)), reads=[w, scT], writes=[pm])
                            op("dve", lambda e: e.tensor_tensor(out=mrow[r][0:1, nb * 512:(nb + 1) * 512], in0=pm[0:1, :], in1=brow[0:1, nb * 512:(nb + 1) * 512], op=ALU.add), reads=[pm, brow], writes=[mrow[r]])

                    def bcast(dst, row_ap, row_t, width=D):
                        for h0 in range(0, width, 512):
                            n = min(512, width - h0)
                            pb = PF[2 + (h0 // 512) % 2]
                            op("pe", lambda e: e.matmul(pb[:, 0:n], lhsT=ones1[0:1, :], rhs=row_ap[0:1, h0:h0 + n], start=True, stop=True), reads=[row_t, ones1], writes=[pb])
                            op("act", lambda e: e.copy(out=dst[:, h0:h0 + n], in_=pb[:, 0:n]), reads=[pb], writes=[dst])

                    for r in range(2):
                        m = mrow[r]
                        seg = lambda i: m[0:1, i * D:(i + 1) * D]
                        op("dve", lambda e: e.scalar_tensor_tensor(out=vrow[:], in0=seg(1), scalar=1.0, in1=g1row[:], op0=ALU.add, op1=ALU.mult), reads=[m, g1row], writes=[vrow])
                        bcast(V[r]["A1"], vrow, vrow)
                        bcast(V[r]["B1"], seg(0), m)
                        if is_pool:
                            op("dve", lambda e: e.tensor_tensor(out=vrow[:], in0=seg(2), in1=psrow[:], op=ALU.mult), reads=[m, psrow], writes=[vrow])
                            bcast(V[r]["G1"], vrow, vrow)
                        else:
                            bcast(V[r]["G1"], seg(2), m)
                        op("dve", lambda e: e.scalar_tensor_tensor(out=vrow[:], in0=seg(4), scalar=1.0, in1=g2row[:], op0=ALU.add, op1=ALU.mult), reads=[m, g2row], writes=[vrow])
                        bcast(V[r]["A2"], vrow, vrow)
                        bcast(V[r]["B2"], seg(3), m)
                        bcast(V[r]["G2"], seg(5), m)
                    if not is_pool:
                        bcast(GQK, qrow, qrow, 1280)
                        op("dve", lambda e: e.tensor_reduce(out=srow[0:1, 20:22], in_=grow[0:1, :].rearrange("p (a b) -> p a b", b=64), axis=AX.X, op=ALU.max, apply_absolute_value=True), reads=[grow], writes=[srow])
                        op("dve", lambda e: e.scalar_tensor_tensor(out=srow[0:1, 0:1], in0=srow[0:1, 20:21], scalar=-8.0, in1=srow[0:1, 21:22], op0=ALU.mult, op1=ALU.mult), reads=[srow], writes=[srow])
                        op("dve", lambda e: e.tensor_scalar(out=srow[0:1, 1:17], in0=srow[0:1, 1:17], scalar1=srow[0:1, 0:1], scalar2=None, op0=ALU.add), reads=[srow], writes=[srow])
                        pb = PF[2]
                        op("pe", lambda e: e.matmul(pb[:, 0:17], lhsT=ones1[0:1, :], rhs=srow[0:1, 0:17], start=True, stop=True), reads=[srow, ones1], writes=[pb])
                        op("dve", lambda e: e.tensor_copy(out=SK[:, 0:1], in_=pb[:, 0:1]), reads=[pb], writes=[SK])
                        op("act", lambda e: e.activation(out=SK[:, 1:17], in_=pb[:, 1:17], func=AF.Exp), reads=[pb], writes=[SK])
                    S.barrier()

                with ExitStack() as esP:
                    d_x = [S.dsem("d_x%d" % i) for i in range(2)]
                    d_o = [S.dsem("d_o%d" % i) for i in range(2)]
                    d_r = [S.dsem("d_r%d" % i) for i in range(2)]
                    d_m = S.dsem("d_m")
                    xt = [sb(esP, "xt%d" % i, [128, D], F32) for i in range(2)]
                    sqj = sb(esP, "sqj", [128, 1280], F32)
                    st1 = sb(esP, "st1", [128, 4], F32)
                    hf = sb(esP, "hf", [128, D], F32)
                    xm = [sb(esP, "xm0", [128, D], F32)] * 2
                    h2f = sb(esP, "h2f", [128, D], F32)
                    h2T = sb(esP, "h2T", [128, 8, 128], F32)
                    rowt = [sb(esP, "rowt%d" % i, [128, RW], BF16) for i in range(2)]
                    lg = sb(esP, "lg", [128, 64], F32)
                    wr = sb(esP, "wr", [128, 8, NE], F32, True)
                    op("sp", lambda e: e.dma_start(out=wr[:], in_=wr_in[l].rearrange("(k p) e -> p k e", p=128)), writes=[wr], dma=d_m)
                    NSLOT = 5
                    slot = lambda j: (j % 3) if j < NT else 3 + (j - NT)
                    if is_pool:
                        hb = [sb(esP, "hb%d" % i, [128, D], BF16) for i in range(NSLOT)]
                        pw = sb(esP, "pw", [128, 8, 256], BF16, True)
                        band = sb(esP, "band", [128, 9, 4, 128], BF16, True)
                        pT = sb(esP, "pT", [128, 8, 128], BF16)
                        with ExitStack() as esW:
                            wtmp = sb(esW, "wtmp", [128, 9 * 4 * 128], F32)
                            op("sp", lambda e: e.dma_start(out=wtmp[:, 0:2048].rearrange("p (k f) -> p k f", f=256), in_=pool_w[jm].rearrange("g (c p) f -> p (g c) f", p=128)), writes=[wtmp], dma=d_m)
                            op("dve", lambda e: e.tensor_copy(out=pw[:].rearrange("p k f -> p (k f)"), in_=wtmp[:, 0:2048]), reads=[wtmp], writes=[pw])
                            op("sp", lambda e: e.dma_start(out=wtmp[:].rearrange("p (v g t) -> p v g t", g=4, t=128), in_=c_band.rearrange("a b p g t -> p (a b) g t")), writes=[wtmp], dma=d_m)
                            op("dve", lambda e: e.tensor_copy(out=band[:].rearrange("p v g t -> p (v g t)"), in_=wtmp[:]), reads=[wtmp], writes=[band])
                            S.barrier()
                    else:
                        kT = [sb(esP, "kT%d" % i, [64, 4, 128], BF16) for i in range(NSLOT)]
                        vv = [sb(esP, "vv%d" % i, [128, 4, 65], BF16) for i in range(NSLOT)]
                        qT = [sb(esP, "qT%d" % i, [64, 16, 128], BF16) for i in range(4)]
                        qslot = lambda j: (j % 2) if j < NT else 2 + (j - NT)
                        wq = sb(esP, "wq", [128, 8, 1536], BF16, True)
                        wob = sb(esP, "wob", [128, 8, D], BF16, True)
                        msk = sb(esP, "msk", [128, 2, 512], BF16, True)
                        hbt = sb(esP, "hbt", [128, D], BF16)
                        hT = sb(esP, "hT", [128, 8, 128], BF16)
                        qkv = sb(esP, "qkv", [128, 1536], F32)
                        qkb = sb(esP, "qkb", [128, 20, 64], BF16)
                        rp = [sb(esP, "rp%d" % i, [128, 20, 32], F32) for i in range(2)]
                        cs = sb(esP, "cs", [128, 64], F32)
                        ptl = [sb(esP, "ptl%d" % i, [128, 512], BF16) for i in range(5)]
                        osb = sb(esP, "osb", [128, D], BF16)
                        oT = sb(esP, "oT", [128, 8, 128], BF16)
                        rden = sb(esP, "rden", [128, 4], F32)
                        for i in range(NSLOT):
                            op("dve", lambda e: e.memset(vv[i][:], 1.0), writes=[vv[i]])
                        with ExitStack() as esW:
                            wtmp = sb(esW, "wtmp", [128, 8, 256], F32)
                            for cb in range(6):
                                op("sp", lambda e: e.dma_start(out=wtmp[:], in_=wqkv[jm, :, cb * 256:(cb + 1) * 256].rearrange("(k p) f -> p k f", p=128)), writes=[wtmp], dma=d_m)
                                op("dve", lambda e: e.tensor_copy(out=wq[:, :, cb * 256:(cb + 1) * 256], in_=wtmp[:]), reads=[wtmp], writes=[wq])
                            for cb in range(4):
                                op("sp", lambda e: e.dma_start(out=wtmp[:], in_=wo[jm, :, cb * 256:(cb + 1) * 256].rearrange("(k p) f -> p k f", p=128)), writes=[wtmp], dma=d_m)
                                op("dve", lambda e: e.tensor_copy(out=wob[:, :, cb * 256:(cb + 1) * 256], in_=wtmp[:]), reads=[wtmp], writes=[wob])
                            wm = wtmp[:].rearrange("p k f -> p (k f)")[:, 0:1024].rearrange("p (m f) -> p m f", m=2)
                            op("sp", lambda e: e.dma_start(out=wm, in_=c_masks.rearrange("m p f -> p m f")), writes=[wtmp], dma=d_m)
                            op("dve", lambda e: e.tensor_copy(out=msk[:], in_=wm), reads=[wtmp], writes=[msk])
                            S.barrier()

                    cnt = {"x": 0}

                    def load_x(j):
                        i = cnt["x"] % 2
                        cnt["x"] += 1
                        t = xt[i]
                        op("sp", lambda e: e.dma_start(out=t[:], in_=X[j * 128:(j + 1) * 128, :]), reads=[bX], writes=[t], dma=d_x[i])
                        return t

                    def norm_mod(src, r, A, B, dst):
                        op("act", lambda e: e.activation(out=sqj[:, 0:D], in_=src[:], func=AF.Square, accum_out=st1[:, 0:1]), reads=[src], writes=[sqj, st1])
                        op("dve", lambda e: e.tensor_scalar(out=st1[:, 1:2], in0=st1[:, 0:1], scalar1=1.0 / D, scalar2=EPS, op0=ALU.mult, op1=ALU.add), reads=[st1], writes=[st1])
                        op("act", lambda e: e.activation(out=st1[:, 2:3], in_=st1[:, 1:2], func=AF.Sqrt), reads=[st1], writes=[st1])
                        op("dve", lambda e: e.reciprocal(out=st1[:, 3:4], in_=st1[:, 2:3]), reads=[st1], writes=[st1])
                        op("dve", lambda e: e.scalar_tensor_tensor(out=dst[:], in0=src[:], scalar=st1[:, 3:4], in1=V[r][A][:], op0=ALU.mult, op1=ALU.mult), reads=[src, st1, V[r][A]], writes=[dst])
                        op("pool", lambda e: e.tensor_tensor(out=dst[:], in0=dst[:], in1=V[r][B][:], op=ALU.add), reads=[dst, V[r][B]], writes=[dst])

                    def stageA(j):
                        r = 0 if j < NT else 1
                        s = slot(j)
                        x = load_x(j)
                        norm_mod(x, r, "A1", "B1", hf)
                        if is_pool:
                            op("act", lambda e: e.copy(out=hb[s][:], in_=hf[:]), reads=[hf], writes=[hb[s]])
                            return
                        op("act", lambda e: e.copy(out=hbt[:], in_=hf[:]), reads=[hf], writes=[hbt])
                        for cc in range(8):
                            op("pe", lambda e: e.transpose(out=PT[:, cc * 128:(cc + 1) * 128], in_=hbt[:, cc * 128:(cc + 1) * 128], identity=identb[:]), reads=[hbt, identb], writes=[PT])
                        op("dve", lambda e: e.tensor_copy(out=hT[:].rearrange("p k t -> p (k t)"), in_=PT[:]), reads=[PT], writes=[hT])
                        for cb in range(3):
                            for kc in range(8):
                                op("pe", lambda e: e.matmul(PF[cb][:], lhsT=hT[:, kc, :], rhs=wq[:, kc, cb * 512:(cb + 1) * 512], start=(kc == 0), stop=(kc == 7)), reads=[hT, wq], writes=[PF[cb]])
                            if cb == 1:
                                op("dve", lambda e: e.tensor_copy(out=qkv[:, cb * 512:(cb + 1) * 512], in_=PF[cb][:]), reads=[PF[cb]], writes=[qkv])
                            else:
                                op("act", lambda e: e.copy(out=qkv[:, cb * 512:(cb + 1) * 512], in_=PF[cb][:]), reads=[PF[cb]], writes=[qkv])
                        op("sp", lambda e: e.dma_start(out=cs[:], in_=c_rope[j * 128:(j + 1) * 128, :]), writes=[cs], dma=d_m)
                        op("act", lambda e: e.activation(out=sqj[:, 0:1280], in_=qkv[:, 0:1280], func=AF.Square), reads=[qkv], writes=[sqj])
                        op("dve", lambda e: e.tensor_reduce(out=lg[:, 0:20], in_=sqj[:, 0:1280].rearrange("p (h d) -> p h d", d=64), axis=AX.X, op=ALU.add), reads=[sqj], writes=[lg])
                        op("dve", lambda e: e.tensor_scalar(out=lg[:, 0:20], in0=lg[:, 0:20], scalar1=1.0 / 64, scalar2=EPS, op0=ALU.mult, op1=ALU.add), reads=[lg], writes=[lg])
                        op("act", lambda e: e.activation(out=lg[:, 0:20], in_=lg[:, 0:20], func=AF.Sqrt), reads=[lg], writes=[lg])
                        op("dve", lambda e: e.reciprocal(out=lg[:, 20:40], in_=lg[:, 0:20]), reads=[lg], writes=[lg])
                        q3 = qkv[:, 0:1280].rearrange("p (h d) -> p h d", d=64)
                        op("dve", lambda e: e.tensor_tensor(out=q3, in0=q3, in1=lg[:, 20:40].unsqueeze(2).to_broadcast([128, 20, 64]), op=ALU.mult), reads=[qkv, lg], writes=[qkv])
                        op("pool", lambda e: e.tensor_tensor(out=qkv[:, 0:1280], in0=qkv[:, 0:1280], in1=GQK[:], op=ALU.mult), reads=[qkv, GQK], writes=[qkv])
                        q4 = qkv[:, 0:1280].rearrange("p (h i two) -> p h i two", two=2, i=32)
                        x0 = q4[:, :, :, 0]
                        x1 = q4[:, :, :, 1]
                        o4 = qkb[:].rearrange("p h (i two) -> p h i two", two=2)
                        cosb = cs[:, 0:32].unsqueeze(1).to_broadcast([128, 20, 32])
                        sinb = cs[:, 32:64].unsqueeze(1).to_broadcast([128, 20, 32])
                        op("dve", lambda e: e.tensor_tensor(out=rp[0][:], in0=x0, in1=cosb, op=ALU.mult), reads=[qkv, cs], writes=[rp[0]])
                        op("pool", lambda e: e.tensor_tensor(out=rp[1][:], in0=x1, in1=sinb, op=ALU.mult), reads=[qkv, cs], writes=[rp[1]])
                        op("dve", lambda e: e.tensor_tensor(out=o4[:, :, :, 0], in0=rp[0][:], in1=rp[1][:], op=ALU.subtract), reads=[rp[0], rp[1]], writes=[qkb])
                        op("dve", lambda e: e.tensor_tensor(out=rp[0][:], in0=x0, in1=sinb, op=ALU.mult), reads=[qkv, cs], writes=[rp[0]])
                        op("pool", lambda e: e.tensor_tensor(out=rp[1][:], in0=x1, in1=cosb, op=ALU.mult), reads=[qkv, cs], writes=[rp[1]])
                        op("dve", lambda e: e.tensor_tensor(out=o4[:, :, :, 1], in0=rp[0][:], in1=rp[1][:], op=ALU.add), reads=[rp[0], rp[1]], writes=[qkb])
                        op("act", lambda e: e.copy(out=vv[s][:, :, 0:64], in_=qkv[:, 1280:1536].rearrange("p (h d) -> p h d", d=64)), reads=[qkv], writes=[vv[s]])
                        for b0 in range(0, 20, 8):
                            nh = min(8, 20 - b0)
                            for hh in range(nh):
                                op("pe", lambda e: e.transpose(out=PT[0:64, hh * 128:(hh + 1) * 128], in_=qkb[:, b0 + hh, :], identity=identb[:]), reads=[qkb, identb], writes=[PT])
                            if b0 < 16:
                                op("dve", lambda e: e.tensor_copy(out=qT[qslot(j)][:, b0:b0 + 8, :].rearrange("p h t -> p (h t)"), in_=PT[0:64, 0:1024]), reads=[PT], writes=[qT[qslot(j)]])
                            else:
                                op("act", lambda e: e.copy(out=kT[s][:].rearrange("p h t -> p (h t)"), in_=PT[0:64, 0:512]), reads=[PT], writes=[kT[s]])

                    def mix_pool(j, x):
                        r = 0 if j < NT else 1
                        n_seq = NT if r == 0 else 2
                        j0 = 0 if r == 0 else NT
                        first = (j == j0)
                        lastt = (j == j0 + n_seq - 1)
                        v = 0 if first else (2 if lastt else 1)
                        srcs = []
                        if not first:
                            srcs.append((0, slot(j - 1)))
                        srcs.append((1, slot(j)))
                        if not lastt:
                            srcs.append((2, slot(j + 1)))
                        for cc in range(8):
                            g = cc // 2
                            pb = PF[cc // 4]
                            for si, (rel, ss) in enumerate(srcs):
                                op("pe", lambda e: e.matmul(pb[:, (cc % 4) * 128:(cc % 4 + 1) * 128], lhsT=hb[ss][:, cc * 128:(cc + 1) * 128], rhs=band[:, v * 3 + rel, g, :], start=(si == 0), stop=(si == len(srcs) - 1)), reads=[hb[ss], band], writes=[pb])
                        op("dve", lambda e: e.tensor_copy(out=pT[:, 0:4, :].rearrange("p k t -> p (k t)"), in_=PF[0][:]), reads=[PF[0]], writes=[pT])
                        op("act", lambda e: e.copy(out=pT[:, 4:8, :].rearrange("p k t -> p (k t)"), in_=PF[1][:]), reads=[PF[1]], writes=[pT])
                        for g in range(4):
                            pb = PF[2 + g // 2]
                            for c2 in range(2):
                                op("pe", lambda e: e.matmul(pb[:, (g % 2) * 256:(g % 2 + 1) * 256], lhsT=pT[:, g * 2 + c2, :], rhs=pw[:, g * 2 + c2, :], start=(c2 == 0), stop=(c2 == 1)), reads=[pT, pw], writes=[pb])
                        return [PF[2], PF[3]]

                    def mix_attn(j):
                        r = 0 if j < NT else 1
                        s = slot(j)
                        chunks = []
                        if r == 0:
                            if j > 0:
                                chunks.append((slot(j - 1), 0))
                            chunks.append((s, None))
                            if j < NT - 1:
                                chunks.append((slot(j + 1), 1))
                        chunks.append((3, None))
                        chunks.append((4, None))
                        for hk in range(4):
                            for ci, (ks, mt) in enumerate(chunks):
                                ps_ = PF[3 + ci % 2]
                                op("pe", lambda e: e.matmul(ps_[:], lhsT=kT[ks][:, hk, :], rhs=qT[qslot(j)][:, hk * 4:(hk + 1) * 4, :].rearrange("p h t -> p (h t)"), start=True, stop=True), reads=[kT[ks], qT[qslot(j)]], writes=[ps_])
                                op("act", lambda e: e.activation(out=ptl[ci][:], in_=ps_[:], func=AF.Exp, scale=0.125, bias=SK[:, 0:1]), reads=[ps_, SK], writes=[ptl[ci]])
                                if mt is not None:
                                    op("pool", lambda e: e.tensor_tensor(out=ptl[ci][:], in0=ptl[ci][:], in1=msk[:, mt, :], op=ALU.mult), reads=[ptl[ci], msk], writes=[ptl[ci]])
                            po = PF[5]
                            for hq in range(4):
                                for ci, (ks, mt) in enumerate(chunks):
                                    op("pe", lambda e: e.matmul(po[:, hq * 65:(hq + 1) * 65], lhsT=ptl[ci][:, hq * 128:(hq + 1) * 128], rhs=vv[ks][:, hk, :], start=(ci == 0), stop=(ci == len(chunks) - 1)), reads=[ptl[ci], vv[ks]], writes=[po])
                            po3 = po[:, 0:260].rearrange("p (h d) -> p h d", d=65)
                            op("dve", lambda e: e.tensor_tensor(out=rden[:], in0=po3[:, :, 64], in1=SK[:, 1 + hk * 4:5 + hk * 4], op=ALU.add), reads=[po, SK], writes=[rden])
                            op("dve", lambda e: e.reciprocal(out=rden[:], in_=rden[:]), reads=[rden], writes=[rden])
                            op("dve", lambda e: e.tensor_tensor(out=osb[:, hk * 256:(hk + 1) * 256].rearrange("p (h d) -> p h d", d=64), in0=po3[:, :, 0:64], in1=rden[:].unsqueeze(2).to_broadcast([128, 4, 64]), op=ALU.mult), reads=[po, rden], writes=[osb])
                        for cc in range(8):
                            op("pe", lambda e: e.transpose(out=PT[:, cc * 128:(cc + 1) * 128], in_=osb[:, cc * 128:(cc + 1) * 128], identity=identb[:]), reads=[osb, identb], writes=[PT])
                        op("dve", lambda e: e.tensor_copy(out=oT[:].rearrange("p k t -> p (k t)"), in_=PT[:]), reads=[PT], writes=[oT])
                        for dh in range(2):
                            for cc in range(8):
                                op("pe", lambda e: e.matmul(PF[dh][:], lhsT=oT[:, cc, :], rhs=wob[:, cc, dh * 512:(dh + 1) * 512], start=(cc == 0), stop=(cc == 7)), reads=[oT, wob], writes=[PF[dh]])
                        return [PF[0], PF[1]]

                    cntb = {"b": 0}

                    def stageB(j):
                        r = 0 if j < NT else 1
                        i = cntb["b"] % 2
                        cntb["b"] += 1
                        x = load_x(j)
                        ys = mix_pool(j, x) if is_pool else mix_attn(j)
                        xmi = xm[i]
                        for dh in range(2):
                            op("dve", lambda e: e.tensor_tensor(out=hf[:, dh * 512:(dh + 1) * 512], in0=ys[dh][:], in1=V[r]["G1"][:, dh * 512:(dh + 1) * 512], op=ALU.mult), reads=[ys[dh], V[r]["G1"]], writes=[hf])
                        op("pool", lambda e: e.tensor_tensor(out=xmi[:], in0=hf[:], in1=x[:], op=ALU.add), reads=[hf, x], writes=[xmi])
                        op("sp", lambda e: e.dma_start(out=X[j * 128:(j + 1) * 128, :], in_=xmi[:]), reads=[xmi], writes=[bX], dma=d_o[i])
                        norm_mod(xmi, r, "A2", "B2", h2f)
                        rt = rowt[i]
                        op("act", lambda e: e.copy(out=rt[:, 0:D], in_=h2f[:]), reads=[h2f], writes=[rt])
                        for cc in range(8):
                            pb = PF[5 + cc // 4]
                            op("pe", lambda e: e.transpose(out=pb[:, (cc % 4) * 128:(cc % 4 + 1) * 128], in_=h2f[:, cc * 128:(cc + 1) * 128], identity=identf[:]), reads=[h2f, identf], writes=[pb])
                        op("dve", lambda e: e.tensor_copy(out=h2T[:, 0:4, :].rearrange("p k t -> p (k t)"), in_=PF[5][:]), reads=[PF[5]], writes=[h2T])
                        op("act", lambda e: e.copy(out=h2T[:, 4:8, :].rearrange("p k t -> p (k t)"), in_=PF[6][:]), reads=[PF[6]], writes=[h2T])
                        for cc in range(8):
                            op("pe", lambda e: e.matmul(PF[6][:, 0:NE], lhsT=h2T[:, cc, :], rhs=wr[:, cc, :], start=(cc == 0), stop=(cc == 7)), reads=[h2T, wr], writes=[PF[6]])
                        op("dve", lambda e: e.tensor_reduce(out=lg[:, 40:41], in_=PF[6][:, 0:NE], axis=AX.X, op=ALU.max), reads=[PF[6]], writes=[lg])
                        op("dve", lambda e: e.tensor_scalar(out=lg[:, 41:42], in0=lg[:, 40:41], scalar1=-1.0, scalar2=None, op0=ALU.mult), reads=[lg], writes=[lg])
                        op("act", lambda e: e.activation(out=lg[:, 44:60], in_=PF[6][:, 0:NE], func=AF.Exp, bias=lg[:, 41:42], accum_out=lg[:, 42:43]), reads=[PF[6], lg], writes=[lg])
                        op("dve", lambda e: e.reciprocal(out=lg[:, 43:44], in_=lg[:, 42:43]), reads=[lg], writes=[lg])
                        op("dve", lambda e: e.tensor_scalar(out=AFF[:, j, :], in0=lg[:, 44:60], scalar1=lg[:, 43:44], scalar2=None, op0=ALU.mult), reads=[lg], writes=[AFF])
                        auxf = rt[:, D:RW].bitcast(F32)
                        auxi = rt[:, D:RW].bitcast(I32)
                        op("dve", lambda e: e.tensor_copy(out=auxf[:, 1:17], in_=AFF[:, j, :]), reads=[AFF], writes=[rt])
                        op("dve", lambda e: e.tensor_copy(out=auxi[:, 0:1], in_=tokid[:, j:j + 1]), reads=[tokid], writes=[rt])
                        op("sp", lambda e: e.dma_start(out=H2[j * 128:(j + 1) * 128, :], in_=rt[:]), reads=[rt], writes=[bH2], dma=d_r[i])

                    for i in range(2):
                        op("dve", lambda e: e.memset(rowt[i][:], 0.0), writes=[rowt[i]])
                    stageA(NT); stageA(NT + 1)
                    if upd_ctx:
                        stageB(NT); stageB(NT + 1)
                    stageA(0)
                    for j in range(NT):
                        if j + 1 < NT:
                            stageA(j + 1)
                        stageB(j)
                    S.barrier()
                    esP.close()

                    with ExitStack() as esR:
                        NJ = [NT, 2]
                        J0 = [0, NT]
                        lo = sb(esR, "lo", [128, 2, NE], F32); hi = sb(esR, "hi", [128, 2, NE], F32); mid = sb(esR, "mid", [128, 2, NE], F32)
                        cvc = sb(esR, "cvc", [128, 2, NE], F32)
                        cmpb = sb(esR, "cmpb", [128, NTT, NE], BF16)
                        part = sb(esR, "part", [128, 2, NE], BF16)
                        fl = sb(esR, "fl", [128, 2, NE], U32); nfl = sb(esR, "nfl", [128, 2, NE], U32)
                        op("dve", lambda e: e.memset(lo[:], 0.0), writes=[lo])
                        op("dve", lambda e: e.memset(hi[:], 1.0), writes=[hi])
                        op("dve", lambda e: e.memset(mid[:], 0.5), writes=[mid])
                        op("dve", lambda e: e.memset(cvc[:, 0, :], float(CL)), writes=[cvc])
                        op("dve", lambda e: e.memset(cvc[:, 1, :], 32.0), writes=[cvc])

                        def compare(th):
                            for s_ in range(2):
                                if s_ == 1 and not upd_ctx:
                                    op("dve", lambda e: e.memset(cmpb[:, NT:NTT, :], 0.0), writes=[cmpb])
                                    continue
                                op("dve", lambda e: e.tensor_tensor(out=cmpb[:, J0[s_]:J0[s_] + NJ[s_], :], in0=AFF[:, J0[s_]:J0[s_] + NJ[s_], :], in1=th[:, s_, :].unsqueeze(1).to_broadcast([128, NJ[s_], NE]), op=ALU.is_ge), reads=[AFF, th], writes=[cmpb])

                        for it in range(34):
                            compare(mid)
                            for s_ in range(2):
                                op("dve", lambda e: e.tensor_reduce(out=part[:, s_, :], in_=cmpb[:, J0[s_]:J0[s_] + NJ[s_], :].rearrange("p j e -> p e j"), axis=AX.X, op=ALU.add), reads=[cmpb], writes=[part])
                            op("pe", lambda e: e.matmul(PF[0][:, 0:32], lhsT=onesb[:], rhs=part[:].rearrange("p s e -> p (s e)"), start=True, stop=True), reads=[part, onesb], writes=[PF[0]])
                            op("dve", lambda e: e.tensor_tensor(out=fl[:].rearrange("p s e -> p (s e)"), in0=PF[0][:, 0:32], in1=cvc[:].rearrange("p s e -> p (s e)"), op=ALU.is_ge), reads=[PF[0], cvc], writes=[fl])
                            op("dve", lambda e: e.tensor_tensor(out=nfl[:].rearrange("p s e -> p (s e)"), in0=PF[0][:, 0:32], in1=cvc[:].rearrange("p s e -> p (s e)"), op=ALU.is_lt), reads=[PF[0], cvc], writes=[nfl])
                            op("dve", lambda e: e.copy_predicated(out=lo[:], mask=fl[:], data=mid[:]), reads=[fl, mid, lo], writes=[lo])
                            op("dve", lambda e: e.copy_predicated(out=hi[:], mask=nfl[:], data=mid[:]), reads=[nfl, mid, hi], writes=[hi])
                            op("dve", lambda e: e.tensor_tensor(out=mid[:], in0=lo[:], in1=hi[:], op=ALU.add), reads=[lo, hi], writes=[mid])
                            op("dve", lambda e: e.tensor_scalar(out=mid[:], in0=mid[:], scalar1=0.5, scalar2=None, op0=ALU.mult), reads=[mid], writes=[mid])
                        compare(lo)
                        M2 = sb(esR, "M2", [128, NTT, NE], F32)
                        CSa = sb(esR, "CSa", [128, NTT, NE], F32)
                        CSb = sb(esR, "CSb", [128, NTT, NE], F32)
                        CS0 = sb(esR, "CS0", [128, NTT, NE], F32)
                        ncol = NTT * NE
                        cm2 = cmpb[:].rearrange("p j e -> p (j e)")
                        for (dst, lt) in ((M2, lstr), (CS0, onesb)):
                            d2 = dst[:].rearrange("p j e -> p (j e)")
                            for c0 in range(0, ncol, 512):
                                n = min(512, ncol - c0)
                                pb = PF[(c0 // 512) % 2]
                                op("pe", lambda e: e.matmul(pb[:, 0:n], lhsT=lt[:], rhs=cm2[:, c0:c0 + n], start=True, stop=True), reads=[cmpb, lt], writes=[pb])
                                op("dve", lambda e: e.tensor_copy(out=d2[:, c0:c0 + n], in_=pb[:, 0:n]), reads=[pb], writes=[dst])
                        op("dve", lambda e: e.tensor_copy(out=CSa[:], in_=CS0[:]), reads=[CS0], writes=[CSa])
                        a, b = CSa, CSb
                        dd = 1
                        while dd < NT:
                            op("dve", lambda e: e.tensor_tensor(out=b[:, dd:NT, :], in0=a[:, dd:NT, :], in1=a[:, 0:NT - dd, :], op=ALU.add), reads=[a], writes=[b])
                            op("pool", lambda e: e.tensor_copy(out=b[:, 0:dd, :], in_=a[:, 0:dd, :]), reads=[a], writes=[b])
                            a, b = b, a
                            dd *= 2
                        op("dve", lambda e: e.tensor_tensor(out=a[:, 0:NT, :], in0=a[:, 0:NT, :], in1=CS0[:, 0:NT, :], op=ALU.subtract), reads=[a, CS0], writes=[a])
                        op("dve", lambda e: e.tensor_tensor(out=M2[:, 0:NT, :], in0=M2[:, 0:NT, :], in1=a[:, 0:NT, :], op=ALU.add), reads=[a, M2], writes=[M2])
                        op("dve", lambda e: e.tensor_tensor(out=M2[:, NT + 1, :], in0=M2[:, NT + 1, :], in1=CS0[:, NT, :], op=ALU.add), reads=[M2, CS0], writes=[M2])
                        op("dve", lambda e: e.tensor_scalar(out=M2[:, NT:NTT, :], in0=M2[:, NT:NTT, :], scalar1=float(CL), scalar2=None, op0=ALU.add), reads=[M2], writes=[M2])
                        op("dve", lambda e: e.tensor_scalar(out=CS0[:], in0=cmpb[:], scalar1=-BIG, scalar2=BIG, op0=ALU.mult, op1=ALU.add), reads=[cmpb], writes=[CS0])
                        op("dve", lambda e: e.tensor_tensor(out=M2[:], in0=M2[:], in1=CS0[:], op=ALU.add), reads=[M2, CS0], writes=[M2])
                        op("dve", lambda e: e.tensor_copy(out=POSI[:], in_=M2[:]), reads=[M2], writes=[POSI])
                        S.barrier()
                esT.close()
                S.barrier()

                with ExitStack() as esE:
                    d_rl = [S.dsem("d_rl%d" % i) for i in range(4)]
                    d_sc = [S.dsem("d_sc%d" % i) for i in range(4)]
                    d_xl = [S.dsem("d_xl%d" % i) for i in range(2)]
                    d_ws = [S.dsem("d_ws%d" % i) for i in range(3)]
                    d_ya = [S.dsem("d_ya%d" % i) for i in range(2)]
                    rring = [sb(esE, "rring%d" % i, [128, RW], BF16) for i in range(3)]
                    xsl = [sb(esE, "xsl%d" % i, [128, RW], BF16) for i in range(2)]
                    xsT = sb(esE, "xsT", [128, 8, NSL], BF16)
                    hTa = sb(esE, "hTa", [128, 16, NSL], BF16)
                    WGs = [sb(esE, "WGs%d" % i, [128, 8, 256], BF16) for i in range(2)]
                    WUs = [sb(esE, "WUs%d" % i, [128, 8, 256], BF16) for i in range(2)]
                    WD = [sb(esE, "WD%d" % i, [128, 2, D], BF16) for i in range(8)]
                    wstg = [sb(esE, "wstg%d" % i, [128, 2048], F32) for i in range(2)]
                    ysb = [sb(esE, "ysb%d" % i, [128, D], F32) for i in range(2)]
                    sact = [sb(esE, "sact0", [128, 512], F32)] * 2
                    IDX = [sb(esE, "IDX%d" % i, [128, NS], I32) for i in range(2)]
                    VAL = [sb(esE, "VAL%d" % i, [128, NS], F32) for i in range(2)]
                    tiles = list(range(NT)) + ([NT, NT + 1] if upd_ctx else [])
                    cw = {"s": 0, "r": 0, "y": 0, "p": 0}
                    gscr = sb(esE, "gscr", [128, 4], F32)

                    def gate(buf):
                        op("pool", lambda e: e.memset(gscr[:, 0:1], 0.0), writes=[buf, gscr])

                    def compaction(e_):
                        if e_ >= NE:
                            return
                        DEPTH = 2
                        pend = []

                        def scat(j, rr, i):
                            bnd = reg_lat if j < NT else reg_ctx
                            op("pool", lambda e: e.indirect_dma_start(out=XS[e_ % 3][:, :], out_offset=bass.IndirectOffsetOnAxis(ap=POSI[:, j, e_:e_ + 1], axis=0), in_=rr[:], in_offset=None, bounds_check=bnd, oob_is_err=False), reads=[rr, POSI, bXS[e_ % 3]], writes=[], dma=d_sc[i])

                        gate(bXS[e_ % 3])
                        for j in tiles:
                            i = cw["r"] % 3
                            cw["r"] += 1
                            rr = rring[i]
                            op("sp", lambda e: e.dma_start(out=rr[:], in_=H2[j * 128:(j + 1) * 128, :]), reads=[bH2], writes=[rr], dma=d_rl[i])
                            pend.append((j, rr, i))
                            if len(pend) > DEPTH:
                                scat(*pend.pop(0))
                            yield
                        while pend:
                            scat(*pend.pop(0))

                    def gu_dma(gp):
                        e2, pc = gp // 8, gp % 8
                        if e2 >= NE:
                            return
                        sg = wstg[0][:].rearrange("p (a b) -> p a b", a=8)
                        su = wstg[1][:].rearrange("p (a b) -> p a b", a=8)
                        op("sp", lambda e: e.dma_start(out=sg, in_=wg_in[l, e2, :, pc * 256:(pc + 1) * 256].rearrange("(k p) f -> p k f", p=128)), writes=[wstg[0]], dma=d_ws[0])
                        op("sp", lambda e: e.dma_start(out=su, in_=wu_in[l, e2, :, pc * 256:(pc + 1) * 256].rearrange("(k p) f -> p k f", p=128)), writes=[wstg[1]], dma=d_ws[1])

                    def gu_cast(gp):
                        e2 = gp // 8
                        if e2 >= NE:
                            return
                        sg = wstg[0][:].rearrange("p (a b) -> p a b", a=8)
                        su = wstg[1][:].rearrange("p (a b) -> p a b", a=8)
                        op("act", lambda e: e.copy(out=WGs[gp % 2][:], in_=sg), reads=[wstg[0]], writes=[WGs[gp % 2]])
                        op("dve", lambda e: e.tensor_copy(out=WUs[gp % 2][:], in_=su), reads=[wstg[1]], writes=[WUs[gp % 2]])

                    def wd_dma(gp):
                        e2, pc = gp // 8, gp % 8
                        if e2 >= NE:
                            return
                        op("pool", lambda e: e.dma_start(out=WD[pc][:], in_=wd_in[l, e2, pc * 256:(pc + 1) * 256, :].rearrange("(c p) d -> p c d", p=128)), writes=[WD[pc]], dma=d_ws[2])

                    def E2(e_):
                        par = e_ % 2
                        gate(bXS[e_ % 3])
                        for st_i in range(NS):
                            xl = xsl[st_i % 2]
                            op("sp", lambda e: e.dma_start(out=xl[:], in_=XS[e_ % 3][st_i * 128:(st_i + 1) * 128, :]), reads=[bXS[e_ % 3]], writes=[xl], dma=d_xl[st_i % 2])
                            op("pool", lambda e: e.tensor_copy(out=IDX[par][:, st_i:st_i + 1], in_=xl[:, D:RW].bitcast(I32)[:, 0:1]), reads=[xl], writes=[IDX[par]])
                            op("pool", lambda e: e.tensor_copy(out=VAL[par][:, st_i:st_i + 1], in_=xl[:, D:RW].bitcast(F32)[:, 1 + e_:2 + e_]), reads=[xl], writes=[VAL[par]])
                            for cc in range(8):
                                op("pe", lambda e: e.transpose(out=PT[:, cc * 128:(cc + 1) * 128], in_=xl[:, cc * 128:(cc + 1) * 128], identity=identb[:]), reads=[xl, identb], writes=[PT])
                            op("dve", lambda e: e.tensor_copy(out=xsT[:, :, st_i * 128:(st_i + 1) * 128], in_=PT[:].rearrange("p (k t) -> p k t", t=128)), reads=[PT], writes=[xsT])

                    for ee in range(2):
                        for _ in compaction(ee):
                            pass
                    gu_dma(0)
                    gu_cast(0)
                    gu_dma(1)
                    E2(0)
                    for e_ in range(NE):
                        par = e_ % 2
                        gen = compaction(e_ + 2)
                        for pc in range(8):
                            gp = e_ * 8 + pc
                            wg_t = WGs[gp % 2]; wu_t = WUs[gp % 2]
                            it_ = 0
                            n_it = len(TT) * 2
                            for ti, (s0_, n) in enumerate(TT):
                                for f2 in range(2):
                                    if it_ == max(n_it - 3, 0):
                                        gu_cast(gp + 1)
                                        gu_dma(gp + 2)
                                        wd_dma(gp)
                                    it_ += 1
                                    fc = pc * 2 + f2
                                    k_ = cw["p"] % 2
                                    cw["p"] += 1
                                    pa = PF[k_ * 2]; pu = PF[k_ * 2 + 1]
                                    for kc in range(8):
                                        op("pe", lambda e: e.matmul(pa[:, 0:n], lhsT=wg_t[:, kc, f2 * 128:(f2 + 1) * 128], rhs=xsT[:, kc, s0_:s0_ + n], start=(kc == 0), stop=(kc == 7)), reads=[wg_t, xsT], writes=[pa])
                                    for kc in range(8):
                                        op("pe", lambda e: e.matmul(pu[:, 0:n], lhsT=wu_t[:, kc, f2 * 128:(f2 + 1) * 128], rhs=xsT[:, kc, s0_:s0_ + n], start=(kc == 0), stop=(kc == 7)), reads=[wu_t, xsT], writes=[pu])
                                    sa = sact[k_]
                                    op("act", lambda e: e.activation(out=sa[:, 0:n], in_=pa[:, 0:n], func=AF.Silu), reads=[pa], writes=[sa])
                                    op("dve", lambda e: e.tensor_tensor(out=hTa[:, fc, s0_:s0_ + n], in0=sa[:, 0:n], in1=pu[:, 0:n], op=ALU.mult), reads=[sa, pu], writes=[hTa])
                                    next(gen, None)
                                    next(gen, None)
                        for _ in gen:
                            pass
                        if e_ + 1 < NE:
                            E2(e_ + 1)
                        gate(bX)
                        for st_i in range(NS):
                            is_ctx = (st_i == NS - 1)
                            if is_ctx and not upd_ctx:
                                continue
                            yi = cw["y"] % 2
                            yb = ysb[yi]
                            cw["y"] += 1
                            g2 = G2[1] if is_ctx else G2[0]
                            for dh in range(2):
                                py = PF[4 + dh]
                                for fc in range(16):
                                    op("pe", lambda e: e.matmul(py[:], lhsT=hTa[:, fc, st_i * 128:(st_i + 1) * 128], rhs=WD[fc // 2][:, fc % 2, dh * 512:(dh + 1) * 512], start=(fc == 0), stop=(fc == 15)), reads=[hTa, WD[fc // 2]], writes=[py])
                                op("dve", lambda e: e.scalar_tensor_tensor(out=yb[:, dh * 512:(dh + 1) * 512], in0=py[:], scalar=VAL[par][:, st_i:st_i + 1], in1=g2[:, dh * 512:(dh + 1) * 512], op0=ALU.mult, op1=ALU.mult), reads=[py, VAL[par], g2], writes=[yb])
                            npart = 32 if is_ctx else 128
                            op("pool", lambda e: e.indirect_dma_start(out=X[:, :], out_offset=bass.IndirectOffsetOnAxis(ap=IDX[par][0:npart, st_i:st_i + 1], axis=0), in_=yb[0:npart, :], in_offset=None, compute_op=ALU.add), reads=[yb, IDX[par], bX], writes=[], dma=d_ya[yi])
                    S.barrier()
        d_out = S.dsem("d_out")
        CH = 1024
        for r0 in range(0, L, CH):
            n = min(CH, L - r0)
            op("sp", lambda e: e.dma_start(out=out_d[r0:r0 + n, :], in_=X[r0:r0 + n, :]), reads=[bX], writes=[bOut], dma=d_out)
        S._wait("sp", [bOut.last_w])
    return nc


_CACHE = {}


def kernel(x, c, ctx, c_ctx, w_ada, b_ada, norm1_g, norm2_g, pool_w, pool_scale,
           attn_w_qkv, attn_w_o, attn_q_norm, attn_k_norm, attn_sink,
           router_w, exp_w_gate, exp_w_up, exp_w_down):
    def f(a):
        a = np.ascontiguousarray(np.asarray(a), dtype=np.float32)
        if a.shape[0] == 0:
            a = np.zeros((1,) + a.shape[1:], np.float32)
        return a
    x = f(x); c = f(c); ctx = f(ctx); c_ctx = f(c_ctx)
    B, L, _ = x.shape
    depth = w_ada.shape[0]
    n_pool = pool_w.shape[0]; n_attn = attn_w_qkv.shape[0]
    NT = L // 128; NTT = NT + 2
    key = (L, depth)
    if key not in _CACHE:
        _CACHE[key] = build(L, depth, n_pool, n_attn)
    nc = _CACHE[key]
    ident = np.eye(128, dtype=np.float32)
    lstrict = (np.arange(128)[:, None] < np.arange(128)[None, :]).astype(np.float32)
    kk = np.arange(128)[:, None]; qq = np.arange(128)[None, :]
    m0 = np.tile((kk >= qq).astype(np.float32), (1, 4)); m1 = np.tile((kk <= qq).astype(np.float32), (1, 4))
    masks = np.stack([m0, m1], 0)
    band = band_consts()
    tokid = (np.arange(NTT)[None, :] * 128 + np.arange(128)[:, None]).astype(np.int32)
    pos = np.arange(L)
    inv = (10000.0 ** (-np.arange(16, dtype=np.float32) / 16)).astype(np.float32)
    ang = np.concatenate([(pos // 64).astype(np.float32)[:, None] * inv, (pos % 64).astype(np.float32)[:, None] * inv], -1).astype(np.float32)
    rope = np.zeros((NTT * 128, 64), np.float32)
    rope[:L, :32] = np.cos(ang); rope[:L, 32:] = np.sin(ang)
    rope[L:, :32] = 1.0
    gqk = np.concatenate([np.tile(f(attn_q_norm), (1, 16)), np.tile(f(attn_k_norm), (1, 4))], 1)
    shared = {
        "w_ada": f(w_ada), "b_ada": f(b_ada), "norm1_g": f(norm1_g), "norm2_g": f(norm2_g),
        "pool_w": f(pool_w), "pool_scale": f(pool_scale), "attn_w_qkv": f(attn_w_qkv), "attn_w_o": f(attn_w_o),
        "gqk": gqk, "attn_q_norm": f(attn_q_norm), "attn_k_norm": f(attn_k_norm), "attn_sink": f(attn_sink),
        "router_w": f(router_w), "exp_w_gate": f(exp_w_gate), "exp_w_up": f(exp_w_up), "exp_w_down": f(exp_w_down),
        "c_ident": ident, "c_lstrict": lstrict, "c_masks": masks, "c_band": band, "c_tokid": tokid, "c_rope": rope,
    }
    in_maps = []
    for k in range(8):
        b = (k * B) // 8
        cv = np.stack([c[b], c_ctx], -1)
        cvT = np.ascontiguousarray(cv.reshape(8, 128, 2).transpose(1, 0, 2))
        m = dict(shared)
        m.update({"x": x[b], "ctx": ctx[b], "cvT": cvT})
        in_maps.append(m)
    res = run_bass_kernel_spmd(nc, in_maps, core_ids=list(range(8)))
    out = np.stack([np.asarray(res.results[(b * 8) // B]["out"]) for b in range(B)], 0)
    return out.astype(np.float32)
```
